# Optimizing a Trainium2 kernel written in Bass

```python
import jax, jax.numpy as jnp
from jax import lax
import numpy as np


D_MODEL = 1024
BATCH = 16
SEQ = 2048
DEPTH = 1

MEM_LEN = 256

GMLP_WIDTH = 1024
GMLP_GROUPS = 8
GMLP_CHUNK = 128
LRU_WIDTH = 1024
LRU_BLOCKS = 8
LRU_BLOCK_DIM = LRU_WIDTH // LRU_BLOCKS
CONV_WIDTH = 4
LRU_C = 8.0
XA_HEADS = 4
XA_HEAD_DIM = 256
XA_WIDTH = XA_HEADS * XA_HEAD_DIM
N_BRANCHES = 3
IN_COLS = 2 * GMLP_WIDTH + 2 * LRU_WIDTH + XA_WIDTH + N_BRANCHES * D_MODEL
N_EXPERTS = 64
TOP_K = 8
N_GROUPS = 8
TOPK_GROUPS = 4
EXPERT_DIM = 256
SHARED_DIM = 256
ROUTED_SCALE = 2.5
EXPERT_BLOCK = 256
LN_EPS = 1e-5
DN_ALPHA = (2.0 * DEPTH) ** 0.25
DN_BETA = (8.0 * DEPTH) ** -0.25

kernel_name = "hybrid_gmlp_rglru_xattn_moe_block"


def layer_norm(x, g, b):
    xf = x.astype(jnp.float32)
    mu = jnp.mean(xf, axis=-1, keepdims=True)
    var = jnp.mean(jnp.square(xf - mu), axis=-1, keepdims=True)
    return ((xf - mu) * lax.rsqrt(var + LN_EPS)).astype(x.dtype) * g + b


def chunked_spatial_gating(uv, norm_g, norm_b, w_sp, b_sp):
    u, v = jnp.split(jax.nn.gelu(uv), 2, axis=-1)
    v = layer_norm(v, norm_g, norm_b)
    B, S, _ = v.shape
    n_chunks = S // GMLP_CHUNK
    vc = v.reshape(B, n_chunks, GMLP_CHUNK, GMLP_GROUPS, GMLP_WIDTH // GMLP_GROUPS)
    causal = jnp.tril(jnp.ones((GMLP_CHUNK, GMLP_CHUNK), dtype=bool))
    w = jnp.where(causal[None], w_sp, 0)
    mixed = jnp.einsum('gts,bnsgc->bntgc', w, vc) + b_sp.T[None, None, :, :, None]
    return u * mixed.reshape(B, S, GMLP_WIDTH)


def causal_depthwise_conv(x, w, b):
    S = x.shape[1]
    xp = jnp.pad(x, ((0, 0), (CONV_WIDTH - 1, 0), (0, 0)))
    out = b
    for k in range(CONV_WIDTH):
        out = out + w[k] * xp[:, CONV_WIDTH - 1 - k: CONV_WIDTH - 1 - k + S]
    return out


def block_diag_linear(x, w, b):
    B, S, _ = x.shape
    xg = x.reshape(B, S, LRU_BLOCKS, LRU_BLOCK_DIM)
    return jnp.einsum('bsgi,gio->bsgo', xg, w).reshape(B, S, LRU_WIDTH) + b


def rg_lru(x, w_a, b_a, w_i, b_i, lam):
    r = jax.nn.sigmoid(block_diag_linear(x, w_a, b_a))
    i = jax.nn.sigmoid(block_diag_linear(x, w_i, b_i))
    log_a = -LRU_C * r.astype(jnp.float32) * jax.nn.softplus(-lam.astype(jnp.float32))
    a = jnp.exp(log_a)
    mult = jnp.sqrt(-jnp.expm1(2.0 * log_a))
    b_term = mult * (i * x).astype(jnp.float32)

    def combine(c1, c2):
        a1, b1 = c1
        a2, b2 = c2
        return a1 * a2, a2 * b1 + b2

    _, h = lax.associative_scan(combine, (a, b_term), axis=1)
    return h.astype(x.dtype)


def memory_cross_attention(q, mem, w_kv):
    B, S, _ = q.shape
    M = mem.shape[1]
    k, v = jnp.split(mem @ w_kv, 2, axis=-1)
    q = q.reshape(B, S, XA_HEADS, XA_HEAD_DIM)
    k = k.reshape(B, M, XA_HEADS, XA_HEAD_DIM)
    v = v.reshape(B, M, XA_HEADS, XA_HEAD_DIM)
    scores = jnp.einsum('bshd,bmhd->bhsm', q, k).astype(jnp.float32) * (XA_HEAD_DIM ** -0.5)
    p = jax.nn.softmax(scores, axis=-1).astype(v.dtype)
    return jnp.einsum('bhsm,bmhd->bshd', p, v).reshape(B, S, XA_WIDTH)


def route(h, w_router, router_bias):
    T = h.shape[0]
    s = jax.nn.sigmoid((h @ w_router).astype(jnp.float32))
    choice = s + router_bias.astype(jnp.float32)
    grp = choice.reshape(T, N_GROUPS, N_EXPERTS // N_GROUPS)
    grp_score = jnp.sum(lax.top_k(grp, 2)[0], axis=-1)
    _, top_grp = lax.top_k(grp_score, TOPK_GROUPS)
    grp_mask = jnp.any(top_grp[:, :, None] == jnp.arange(N_GROUPS)[None, None, :], axis=1)
    exp_mask = jnp.repeat(grp_mask, N_EXPERTS // N_GROUPS, axis=-1)
    _, idx = lax.top_k(jnp.where(exp_mask, choice, -jnp.inf), TOP_K)
    w = jnp.take_along_axis(s, idx, axis=-1)
    w = w / jnp.sum(w, axis=-1, keepdims=True) * ROUTED_SCALE
    return idx, w


def routed_experts(h, idx, wts, w_gate, w_up, w_down):
    T, D = h.shape
    A = T * TOP_K
    n_blocks = -(-A // EXPERT_BLOCK) + N_EXPERTS
    P = n_blocks * EXPERT_BLOCK
    flat_e = idx.reshape(A)
    flat_tok = jnp.arange(A, dtype=jnp.int32) // TOP_K
    flat_w = wts.reshape(A)
    order = jnp.argsort(flat_e)
    sorted_e = flat_e[order]
    counts = jnp.bincount(flat_e, length=N_EXPERTS)
    padded = (counts + EXPERT_BLOCK - 1) // EXPERT_BLOCK * EXPERT_BLOCK
    pad_end = jnp.cumsum(padded)
    pad_start = pad_end - padded
    start = jnp.cumsum(counts) - counts
    dest = pad_start[sorted_e] + (jnp.arange(A, dtype=jnp.int32) - start[sorted_e])
    row_tok = jnp.zeros((P,), jnp.int32).at[dest].set(flat_tok[order])
    row_w = jnp.zeros((P,), wts.dtype).at[dest].set(flat_w[order])
    block_start = jnp.arange(n_blocks, dtype=jnp.int32) * EXPERT_BLOCK
    block_e = jnp.minimum(jnp.searchsorted(pad_end, block_start, side='right'), N_EXPERTS - 1)

    def body(acc, blk):
        tok, rw, e = blk
        xb = h[tok]
        yb = (jax.nn.silu(xb @ w_gate[e]) * (xb @ w_up[e])) @ w_down[e]
        return acc.at[tok].add(yb * rw[:, None].astype(yb.dtype)), None

    acc, _ = lax.scan(body, jnp.zeros_like(h),
                      (row_tok.reshape(n_blocks, EXPERT_BLOCK),
                       row_w.reshape(n_blocks, EXPERT_BLOCK), block_e))
    return acc


def hybrid_layer(h, mem, w_in, gmlp_norm_g, gmlp_norm_b, gmlp_spatial_w, gmlp_spatial_b,
                 conv_w, conv_b, lru_wa, lru_ba, lru_wi, lru_bi, lru_lambda, w_kv,
                 w_br_gmlp, w_br_lru, w_br_xa, w_out, ln1_g, ln1_b,
                 w_router, router_bias, exp_gate, exp_up, exp_down, sh_gate, sh_up, sh_down, ln2_g, ln2_b):
    B, S, D = h.shape
    z = h @ w_in
    c1 = 2 * GMLP_WIDTH
    c2 = c1 + LRU_WIDTH
    c3 = c2 + LRU_WIDTH
    c4 = c3 + XA_WIDTH
    uv, lru_x, lru_g, q, gates = jnp.split(z, [c1, c2, c3, c4], axis=-1)
    y_gmlp = chunked_spatial_gating(uv, gmlp_norm_g, gmlp_norm_b, gmlp_spatial_w, gmlp_spatial_b)
    xc = causal_depthwise_conv(lru_x, conv_w, conv_b)
    y_lru = rg_lru(xc, lru_wa, lru_ba, lru_wi, lru_bi, lru_lambda) * jax.nn.gelu(lru_g)
    y_xa = memory_cross_attention(q, mem, w_kv)
    g = jax.nn.sigmoid(gates).reshape(B, S, N_BRANCHES, D)
    merged = (g[:, :, 0] * (y_gmlp @ w_br_gmlp)
              + g[:, :, 1] * (y_lru @ w_br_lru)
              + g[:, :, 2] * (y_xa @ w_br_xa))
    h = layer_norm(DN_ALPHA * h + merged @ w_out, ln1_g, ln1_b)
    hf = h.reshape(B * S, D)
    idx, wts = route(hf, w_router, router_bias)
    routed = routed_experts(hf, idx, wts, exp_gate, exp_up, exp_down)
    shared = (jax.nn.silu(hf @ sh_gate) * (hf @ sh_up)) @ sh_down
    return layer_norm(DN_ALPHA * h + (routed + shared).reshape(B, S, D), ln2_g, ln2_b)


def setup_inputs(seed: int = 0) -> dict:
    key = jax.random.key(seed)
    ks = iter(jax.random.split(key, 48))
    f32 = jnp.float32
    L, D = DEPTH, D_MODEL

    def nrm(shape, scale):
        return jax.random.normal(next(ks), shape, f32) * scale

    u = jax.random.uniform(next(ks), (L, LRU_WIDTH), f32, 0.9, 0.999)
    p = u ** (1.0 / LRU_C)
    lam = jnp.log(p) - jnp.log1p(-p)
    w_kv = jnp.concatenate([nrm((L, D, XA_WIDTH), D ** -0.5),
                            nrm((L, D, XA_WIDTH), DN_BETA * D ** -0.5)], axis=-1)
    return {
        'x': nrm((BATCH, SEQ, D), 1.0),
        'mem': nrm((BATCH, MEM_LEN, D), 1.0),
        'ln_in_g': 1.0 + nrm((D,), 0.02),
        'ln_in_b': nrm((D,), 0.02),
        'w_in': nrm((L, D, IN_COLS), D ** -0.5),
        'gmlp_norm_g': 1.0 + nrm((L, GMLP_WIDTH), 0.02),
        'gmlp_norm_b': nrm((L, GMLP_WIDTH), 0.02),
        'gmlp_spatial_w': nrm((L, GMLP_GROUPS, GMLP_CHUNK, GMLP_CHUNK), GMLP_CHUNK ** -0.5),
        'gmlp_spatial_b': 1.0 + nrm((L, GMLP_GROUPS, GMLP_CHUNK), 0.1),
        'conv_w': nrm((L, CONV_WIDTH, LRU_WIDTH), CONV_WIDTH ** -0.5),
        'conv_b': nrm((L, LRU_WIDTH), 0.02),
        'lru_wa': nrm((L, LRU_BLOCKS, LRU_BLOCK_DIM, LRU_BLOCK_DIM), LRU_BLOCK_DIM ** -0.5),
        'lru_ba': nrm((L, LRU_WIDTH), 0.02),
        'lru_wi': nrm((L, LRU_BLOCKS, LRU_BLOCK_DIM, LRU_BLOCK_DIM), LRU_BLOCK_DIM ** -0.5),
        'lru_bi': nrm((L, LRU_WIDTH), 0.02),
        'lru_lambda': lam,
        'w_kv': w_kv,
        'w_br_gmlp': nrm((L, GMLP_WIDTH, D), DN_BETA * GMLP_WIDTH ** -0.5),
        'w_br_lru': nrm((L, LRU_WIDTH, D), DN_BETA * LRU_WIDTH ** -0.5),
        'w_br_xa': nrm((L, XA_WIDTH, D), DN_BETA * XA_WIDTH ** -0.5),
        'w_out': nrm((L, D, D), DN_BETA * D ** -0.5),
        'ln1_g': 1.0 + nrm((L, D), 0.02),
        'ln1_b': nrm((L, D), 0.02),
        'w_router': nrm((L, D, N_EXPERTS), D ** -0.5),
        'router_bias': nrm((L, N_EXPERTS), 0.01),
        'exp_gate': nrm((L, N_EXPERTS, D, EXPERT_DIM), DN_BETA * D ** -0.5),
        'exp_up': nrm((L, N_EXPERTS, D, EXPERT_DIM), DN_BETA * D ** -0.5),
        'exp_down': nrm((L, N_EXPERTS, EXPERT_DIM, D), DN_BETA * EXPERT_DIM ** -0.5),
        'sh_gate': nrm((L, D, SHARED_DIM), DN_BETA * D ** -0.5),
        'sh_up': nrm((L, D, SHARED_DIM), DN_BETA * D ** -0.5),
        'sh_down': nrm((L, SHARED_DIM, D), DN_BETA * SHARED_DIM ** -0.5),
        'ln2_g': 1.0 + nrm((L, D), 0.02),
        'ln2_b': nrm((L, D), 0.02),
    }


def reference(x, mem, ln_in_g, ln_in_b, w_in, gmlp_norm_g, gmlp_norm_b, gmlp_spatial_w, gmlp_spatial_b,
              conv_w, conv_b, lru_wa, lru_ba, lru_wi, lru_bi, lru_lambda, w_kv,
              w_br_gmlp, w_br_lru, w_br_xa, w_out, ln1_g, ln1_b,
              w_router, router_bias, exp_gate, exp_up, exp_down, sh_gate, sh_up, sh_down, ln2_g, ln2_b):
    h = layer_norm(x, ln_in_g, ln_in_b)
    for l in range(DEPTH):
        h = hybrid_layer(h, mem, w_in[l], gmlp_norm_g[l], gmlp_norm_b[l], gmlp_spatial_w[l], gmlp_spatial_b[l],
                         conv_w[l], conv_b[l], lru_wa[l], lru_ba[l], lru_wi[l], lru_bi[l], lru_lambda[l], w_kv[l],
                         w_br_gmlp[l], w_br_lru[l], w_br_xa[l], w_out[l], ln1_g[l], ln1_b[l],
                         w_router[l], router_bias[l], exp_gate[l], exp_up[l], exp_down[l],
                         sh_gate[l], sh_up[l], sh_down[l], ln2_g[l], ln2_b[l])
    return h
```

```python
import bisect
from contextlib import ExitStack

import numpy as np
import concourse.bass as bass
import concourse.mybir as mybir
from concourse.bass_utils import run_bass_kernel_spmd

F32 = mybir.dt.float32
BF16 = mybir.dt.bfloat16
AF = mybir.ActivationFunctionType
ALU = mybir.AluOpType
AX = mybir.AxisListType

COMPUTE = ("pe", "act", "dve", "pool")
ENGS = ("pe", "act", "dve", "pool", "sp")

NCORES = 8
D = 1024
SEQ = 2048
TOK = 2 * SEQ
T = 256
NT = TOK // T
LN_EPS = 1e-5
ALPHA = 2.0 ** 0.25
NEXP = 64
CAP = 896
NSLOT = NEXP * CAP


class Buf:
    __slots__ = ("name", "w", "r", "excl")

    def __init__(self, name, excl=False):
        self.name = name
        self.w = None
        self.r = {}
        self.excl = excl


class Sched:
    def __init__(self):
        self.ops = {e: [] for e in ENGS}
        self.cnt = {e: 0 for e in COMPUTE}
        self.waited = {e: {} for e in ENGS}
        self.dma_cnt = {}
        self.pending = {e: [] for e in ENGS}

    def barrier(self):
        evs = [(e, self.cnt[e]) for e in COMPUTE if self.cnt[e] > 0]
        evs += [(k, v) for k, v in self.dma_cnt.items()]
        for e in ENGS:
            self.pending[e] = list(evs)

    def op(self, eng, fn, reads=(), writes=(), dma=None):
        waits = []
        wd = self.waited[eng]

        def need(ev, raw):
            if ev is None:
                return
            key, val = ev
            if key == eng and dma is None:
                if eng == "pe":
                    return
            if wd.get(key, 0) >= val:
                return
            wd[key] = val
            waits.append((key, val))

        if self.pending[eng]:
            for ev in self.pending[eng]:
                need(ev, True)
            self.pending[eng] = []
        for b in reads:
            need(b.w, True)
            if b.excl:
                for k, v in b.r.items():
                    need((k, v), False)
        for b in writes:
            need(b.w, False)
            for k, v in b.r.items():
                need((k, v), False)
        if dma is None:
            self.cnt[eng] += 1
            ev = (eng, self.cnt[eng])
        else:
            self.dma_cnt[dma] = self.dma_cnt.get(dma, 0) + 16
            ev = (dma, self.dma_cnt[dma])
        for b in writes:
            b.w = ev
            b.r = {}
        for b in reads:
            if b.r.get(ev[0], 0) < ev[1]:
                b.r[ev[0]] = ev[1]
        self.ops[eng].append((waits, fn, ev))
        return ev

    def sem_keys(self):
        return list(COMPUTE) + sorted(self.dma_cnt.keys())

    def emit(self, nc, sems, final_waits=()):
        sig = {e: set() for e in COMPUTE}
        for eng in ENGS:
            for waits, fn, ev in self.ops[eng]:
                for k, v in waits:
                    if k in sig:
                        sig[k].add(v)
        sigl = {e: sorted(sig[e]) for e in COMPUTE}

        def val(k, v):
            if k in sigl:
                return bisect.bisect_left(sigl[k], v) + 1
            return v

        with nc.Block() as block:
            def run(eng_name, e):
                for waits, fn, ev in self.ops[eng_name]:
                    for k, v in waits:
                        e.wait_ge(sems[k], val(k, v))
                    ins = fn(e)
                    if ev[0] in sig:
                        if ev[1] in sig[ev[0]]:
                            ins.then_inc(sems[ev[0]], 1)
                    else:
                        ins.then_inc(sems[ev[0]], 16)
                if eng_name == "sp":
                    for k, v in final_waits:
                        e.wait_ge(sems[k], v)

            @block.tensor
            def _(e):
                run("pe", e)

            @block.scalar
            def _(e):
                run("act", e)

            @block.vector
            def _(e):
                run("dve", e)

            @block.gpsimd
            def _(e):
                run("pool", e)

            @block.sync
            def _(e):
                run("sp", e)


def build(n_tiles=NT, n_exp=NEXP + 1, debug=False):
    nc = bass.Bass("TRN2", target_bir_lowering=False)
    S = Sched()

    def din(name, shape, dt=F32):
        return nc.dram_tensor(name, shape, dt, kind="ExternalInput").ap()

    x_d = din("x", [TOK, D])
    mem_d = din("mem", [512, D])
    lnvec_d = din("lnvec", [128, 8, D])
    cvec_d = din("cvec", [128, 64])
    wspT_d = din("wspT", [128, 8, 128])
    maskT_d = din("maskT", [128, 128])
    bsp_d = din("bsp", [1, 1024])
    wa_d = din("wa", [128, 8, 128])
    wi_d = din("wi", [128, 8, 128])
    wr_d = din("wr", [128, 8, 64])
    rbias_d = din("rbias", [128, 64])
    ident_d = din("ident", [128, 128])
    iotaC_d = din("iotaC", [128, 64])
    ustrict_d = din("ustrict", [128, 128])
    dummy_d = din("dummyidx", [128, 1])
    kvals_d = din("kvals", [128, 8])
    w_in_d = din("w_in", [D, 8192])
    w_kv_d = din("w_kv", [D, 2048])
    w_br_d = [din(f"w_br{k}", [D, D]) for k in range(3)]
    w_out_d = din("w_out", [D, D])
    eg_d = din("exp_gate", [NEXP, D, 256])
    eu_d = din("exp_up", [NEXP, D, 256])
    ed_d = din("exp_down", [NEXP, 256, D])
    sg_d = din("sh_gate", [D, 256])
    su_d = din("sh_up", [D, 256])
    sd_d = din("sh_down", [256, D])
    out_d = nc.dram_tensor("out", [TOK, D], F32, kind="ExternalOutput").ap()
    wsc = nc.dram_tensor("wsc", [28, 128, 4096], BF16).ap()
    dk = dict(kind="ExternalOutput") if debug else {}
    r1s = nc.dram_tensor("r1s", [TOK, D], F32, **dk).ap()
    h1Ts = nc.dram_tensor("h1Ts", [2, 128, 8 * SEQ], BF16, **dk).ap()
    Xs = nc.dram_tensor("Xs", [NSLOT + 128, D], BF16).ap()
    Ys = nc.dram_tensor("Ys", [NSLOT + 128, D], BF16).ap()
    ews = nc.dram_tensor("ews", [NEXP + 1, 128, 6144], BF16).ap()
    if debug:
        dbg_wden = nc.dram_tensor("dbg_wden", [128, 32, 8], F32, kind="ExternalOutput").ap()
        dbg_dst = nc.dram_tensor("dbg_dst", [128, 32, 8], mybir.dt.uint32, kind="ExternalOutput").ap()
        dbg_y = nc.dram_tensor("dbg_y", [4, 128, 8 * T], BF16, kind="ExternalOutput").ap()

    es = ExitStack()
    with es:
        def sb(name, shape, dt):
            return es.enter_context(nc.sbuf_tensor("s_" + name, shape, dt))

        FSZ = 24848
        BSZ = 31360
        Freg = sb("Freg", [128, FSZ], F32)
        Breg = sb("Breg", [128, BSZ], BF16)
        wsl = sb("wsl", [128, 4, 4096], BF16)
        wden = sb("wden", [128, 1, 64], F32)
        ident_bf = sb("ident_bf", [128, 128], BF16)
        ustr_bf = sb("ustr_bf", [128, 128], BF16)
        iotaC = sb("iotaC", [128, 64], F32)
        dummy = sb("dummy", [128, 1], F32)
        kvals = sb("kvals", [128, 8], F32)
        carry = sb("carry", [128, 64], F32)
        ones64 = sb("ones64", [128, 64], F32)
        dst_all = sb("dst_all", [128, 32, 8], mybir.dt.uint32)
        wk_all = sb("wk_all", [128, 32, 8], F32)
        ident = sb("ident", [128, 128], F32)
        ones_bf = sb("ones_bf", [128, 128], BF16)
        wspT = sb("wspT", [128, 8, 128], BF16)
        bspf = sb("bspf", [128, 1024], BF16)
        wa = sb("wa", [128, 8, 128], BF16)
        wi = sb("wi", [128, 8, 128], BF16)
        wr = sb("wr", [128, 8, 64], F32)
        rbias = sb("rbias", [128, 64], F32)
        cvec = sb("cvec", [128, 64], F32)
        cch = sb("cch", [128, 8], F32)
        chf = sb("chf", [128, 24], F32)
        cpow = sb("cpow", [128, 2], F32)

        halo = sb("halo", [128, 8, 4], F32)
        hstate = sb("hstate", [128, 8], F32)
        pb = [es.enter_context(nc.psum_tensor(f"pb{i}", [128, 512], F32)) for i in range(8)]
        Pb = [Buf(f"pb{i}", True) for i in range(8)]
        bank_ctr = [0]

        pools = {"C": [0, 1, 2, 3], "D": [4, 5], "S": [6, 7]}
        pool_ctr = {k: 0 for k in pools}
        cur_pool = [None]

        def nb(pool=None):
            pool = pool or cur_pool[0]
            if pool is None:
                i = bank_ctr[0] % 8
                bank_ctr[0] += 1
                return i
            lst = pools[pool]
            i = lst[pool_ctr[pool] % len(lst)]
            pool_ctr[pool] += 1
            return i

        def MM(out, lhsT, rhs, start, stop, reads, bank):
            S.op("pe", lambda e: e.matmul(out, lhsT=lhsT, rhs=rhs, start=start, stop=stop),
                 reads=reads, writes=[Pb[bank]])

        b_ident = Buf("ident")

        def TR(out, in_, reads, bank):
            S.op("pe", lambda e: e.transpose(out=out, in_=in_, identity=ident[:]),
                 reads=list(reads) + [b_ident], writes=[Pb[bank]])

        def ACT(out, in_, func, reads, writes, bias=None, scale=None):
            kw = {}
            if bias is not None:
                kw["bias"] = bias
            if scale is not None:
                kw["scale"] = scale
            S.op("act", lambda e: e.activation(out=out, in_=in_, func=func, **kw), reads=reads, writes=writes)

        def TS(eng, out, in0, s1, s2, op0, op1, reads, writes):
            if s2 is None:
                S.op(eng, lambda e: e.tensor_scalar(out=out, in0=in0, scalar1=s1, scalar2=None, op0=op0),
                     reads=reads, writes=writes)
            else:
                S.op(eng, lambda e: e.tensor_scalar(out=out, in0=in0, scalar1=s1, scalar2=s2, op0=op0, op1=op1),
                     reads=reads, writes=writes)

        def TT(eng, out, in0, in1, op, reads, writes):
            S.op(eng, lambda e: e.tensor_tensor(out=out, in0=in0, in1=in1, op=op), reads=reads, writes=writes)

        def STT(out, in0, scalar, in1, op0, op1, reads, writes):
            S.op("dve", lambda e: e.scalar_tensor_tensor(out=out, in0=in0, scalar=scalar, in1=in1, op0=op0, op1=op1),
                 reads=reads, writes=writes)

        def CP(eng, out, in_, reads, writes):
            if eng == "act":
                S.op(eng, lambda e: e.copy(out=out, in_=in_), reads=reads, writes=writes)
            else:
                S.op(eng, lambda e: e.tensor_copy(out=out, in_=in_), reads=reads, writes=writes)

        def MEMSET(eng, ap, v, writes):
            S.op(eng, lambda e: e.memset(ap, v), writes=writes)

        def DMA(eng, out, in_, reads, writes, key):
            S.op(eng, lambda e: e.dma_start(out=out, in_=in_), reads=reads, writes=writes, dma=key)

        foff = [0]
        boff = [0]

        def fr(n):
            o = foff[0]
            foff[0] += n
            assert foff[0] <= FSZ, foff[0]
            return Freg[:, o:o + n]

        def br(n):
            o = boff[0]
            boff[0] += n
            assert boff[0] <= BSZ, boff[0]
            return Breg[:, o:o + n]

        lnv = fr(6 * 1024).rearrange("p (a d) -> p a d", a=6)
        b_lnv = Buf("lnv")
        HX = [[fr(1024) for _ in range(2)] for _ in range(2)]
        b_HX = [[Buf(f"hx{p}{s}") for s in range(2)] for p in range(2)]
        xin = HX[0]
        b_xin = b_HX[0]
        vg = [fr(1024) for _ in range(2)]
        b_vg = [Buf(f"vg{s}") for s in range(2)]
        lru = []
        mh4 = fr(1024)
        for par in range(4):
            d_ = dict(xl=fr(260), xc=fr(256), ra=fr(256), ib=fr(256), mh=mh4[:, par * 256:(par + 1) * 256])
            d_["B"] = {k: Buf(f"lru{par}{k}") for k in ("xl", "xc", "ra", "ib", "mh")}
            lru.append(d_)
        b_glb = [Buf(f"glb{i}") for i in range(8)]
        sig = [fr(256) for _ in range(2)]
        b_sig = [Buf(f"sig{k}") for k in range(2)]
        prod = [fr(256) for _ in range(2)]
        b_prod = [Buf(f"prod{k}") for k in range(2)]
        macc_off = foff[0]
        macc = [fr(256) for _ in range(4)]
        b_macc = [Buf(f"macc{k}") for k in range(4)]
        rden = fr(256)
        b_rden = Buf("rden")
        r1 = [fr(1024) for _ in range(2)]
        b_r1 = [Buf(f"r1_{i}") for i in range(2)]
        h1t = fr(1024)
        b_h1t = Buf("h1t")
        h1Tf = fr(1024).rearrange("p (k t) -> p k t", k=8)
        b_h1Tf = Buf("h1Tf")

        def stats_set(name):
            return dict(st=fr(24), mv=fr(4), rs=fr(2), B=Buf(name))
        ss_in = [stats_set(f"ss_in{s}") for s in range(2)]
        ss_v = [stats_set(f"ss_v{s}") for s in range(2)]
        ss_1 = stats_set("ss_1")
        ss_2 = stats_set("ss_2")
        rt = dict(s=fr(64), ch=fr(64), o8=fr(64), gs=fr(8), srt=fr(8), gm=fr(8), nbg=fr(8), msk=fr(64), t8=fr(8),
                  sel=fr(64), wun=fr(64), den=fr(2), rd=fr(2), cnt=fr(64), valid=fr(64), dall=fr(64), wv=fr(64),
                  csum=fr(64), csel=fr(64), dstf=fr(8))
        rt["oh"] = vg[0][:, 0:512]
        rt["pr"] = vg[0][:, 512:1024]
        b_rt = {k: Buf("rt_" + k) for k in rt}
        f_mixer_end = foff[0]

        h0Tp = [br(8 * T).rearrange("p (k t) -> p k t", k=8) for _ in range(2)]
        b_h0Tp = [Buf(f"h0T{p}") for p in range(2)]
        ybr0p = [br(8 * T).rearrange("p (k t) -> p k t", k=8) for _ in range(2)]
        b_ybr0p = [[Buf(f"y0{p}_{c}") for c in range(8)] for p in range(2)]
        glb = [br(T) for _ in range(8)]
        vn = br(2 * 1024).rearrange("p (s d) -> p s d", s=2)
        b_vn = [Buf(f"vn{s}") for s in range(2)]
        ub = [br(T) for _ in range(2)]
        b_ub = [Buf(f"u{i}") for i in range(2)]
        ybr = [None] + [br(8 * T).rearrange("p (k t) -> p k t", k=8) for _ in range(2)]
        b_ybr = [None] + [[Buf(f"y{k}_{c}") for c in range(8)] for k in (1, 2)]
        xcb = [br(T) for _ in range(4)]
        b_xcb = [Buf(f"xcb{i}") for i in range(4)]
        qT = br(8 * T).rearrange("p (k t) -> p k t", k=8)
        b_qT = [Buf(f"qT{c}") for c in range(8)]
        expT = [br(2 * T).rearrange("p (m t) -> p m t", m=2) for _ in range(2)]
        b_expT = [Buf(f"expT{i}") for i in range(2)]
        kT = br(8 * 256).rearrange("p (k t) -> p k t", k=8)
        b_kT = Buf("kT")
        vv = br(2 * 1024).rearrange("p (m d) -> p m d", m=2)
        b_vv = Buf("vv")
        memT = None
        mrg = br(8 * T).rearrange("p (k t) -> p k t", k=8)
        b_mrg = [Buf(f"mrg{c}") for c in range(8)]
        memT = qT
        h1Tb = br(8 * T).rearrange("p (k t) -> p k t", k=8)
        b_h1Tb = Buf("h1Tb")
        h1b = [br(1024) for _ in range(2)]
        b_h1b = [Buf(f"h1b{i}") for i in range(2)]
        selb = br(64)
        b_selb = Buf("selb")
        b_Xs = Buf("Xs")
        b_Ys = Buf("Ys")
        b_ews = Buf("ews")
        b_dst = Buf("dst_all")
        b_wk = Buf("wk_all")
        b_carry = Buf("carry")

        wslv = [wsl[:, i, :].rearrange("p (k c) -> p k c", k=8) for i in range(4)]
        b_wsl = [Buf(f"wsl{i}") for i in range(4)]
        wsl_ctr = [0]
        b_wden = Buf("wden")
        b_ln2v = Buf("ln2v")
        b_const = Buf("const")
        b_halo = Buf("halo")
        b_hst = Buf("hstate")
        b_wsc = Buf("wsc")

        blk_id = {}
        nblk = [0]

        b_wscB = Buf("wscB")
        blk_buf = {}

        pre_bufs = {}

        def precast(name, src, cbs, grp):
            for cb in cbs:
                n = nblk[0]
                blk_id[(name, cb)] = n
                grp_i = n // 4
                if grp_i not in pre_bufs:
                    pre_bufs[grp_i] = Buf(f"wscg{grp_i}")
                bb = pre_bufs[grp_i]
                blk_buf[(name, cb)] = bb
                DMA("pool", wsc[n].rearrange("p (k c) -> p k c", k=8),
                    src[:, cb * 512:(cb + 1) * 512].rearrange("(k p) c -> p k c", p=128), [], [], f"d_pre{grp_i}")
                bb.w = (f"d_pre{grp_i}", S.dma_cnt[f"d_pre{grp_i}"])
                nblk[0] += 1
        precast("kv", w_kv_d, range(4), 0)
        precast("in", w_in_d, [2, 3, 0, 1, 6, 7, 4, 8, 5, 9], 0)
        for jb in range(2):
            for k in range(3):
                precast("in", w_in_d, [10 + 2 * k + jb], 1)
                precast(f"br{k}", w_br_d[k], [jb], 1)
        precast("out", w_out_d, range(2), 1)
        def precast_expert(e):
            if e < NEXP:
                g_src, u_src, d_src = eg_d[e], eu_d[e], ed_d[e]
            else:
                g_src, u_src, d_src = sg_d, su_d, sd_d
            gu = ews[e][:, 0:4096].rearrange("p (k c) -> p k c", k=8)
            DMA("pool", gu[:, :, 0:256], g_src.rearrange("(k p) f -> p k f", p=128), [], [], "d_pre2")
            DMA("pool", gu[:, :, 256:512], u_src.rearrange("(k p) f -> p k f", p=128), [], [], "d_pre2")
            DMA("pool", ews[e][:, 4096:6144].rearrange("p (f d) -> p f d", f=2),
                d_src.rearrange("(f p) d -> p f d", p=128), [], [], "d_pre2")
            b_ews.w = ("d_pre2", S.dma_cnt["d_pre2"])

        def wload(name, cb, slot=None):
            if slot is None:
                i = wsl_ctr[0] % 4
                wsl_ctr[0] += 1
            else:
                i = slot
            DMA("sp", wsl[:, i, :], wsc[blk_id[(name, cb)]], [blk_buf[(name, cb)]], [b_wsl[i]], f"d_wsl{i}")
            return i

        DMA("sp", ident[:], ident_d, [], [b_ident], "d_c0")
        DMA("sp", cvec[:], cvec_d, [], [b_const], "d_c1")
        DMA("sp", wr[:], wr_d, [], [b_const], "d_c1")
        DMA("sp", rbias[:], rbias_d, [], [b_const], "d_c1")
        DMA("sp", lnv, lnvec_d[:, 0:6, :], [], [b_lnv], "d_c3")
        MEMSET("pool", bspf[:], 0.0, [b_const])
        DMA("pool", bspf[0:1, :], bsp_d, [], [b_const], "d_c4")
        DMA("pool", wa[:], wa_d, [], [b_const], "d_c4")
        DMA("pool", wi[:], wi_d, [], [b_const], "d_c4")
        MEMSET("dve", ones_bf[:], 1.0, [b_const])
        MEMSET("dve", ones64[:], 1.0, [b_const])
        MEMSET("dve", carry[:], 0.0, [b_carry])
        DMA("sp", iotaC[:], iotaC_d, [], [b_const], "d_c1")
        DMA("sp", dummy[:], dummy_d, [], [b_const], "d_c1")
        DMA("sp", kvals[:], kvals_d, [], [b_const], "d_c1")
        DMA("pool", ustr_bf[:], ustrict_d, [], [b_const], "d_c4")
        DMA("pool", ident_bf[:], ident_d, [], [b_const], "d_c4")
        MEMSET("pool", h1b[0], 0.0, [b_h1b[0]])
        DMA("sp", Ys[NSLOT:NSLOT + 128, :], h1b[0], [b_h1b[0]], [b_Ys], "d_ysz")
        stg = xin[0].rearrange("p (g t) -> p g t", g=8)
        DMA("sp", stg, wspT_d, [], [b_xin[0]], "d_xin0")
        DMA("sp", xin[1][:, 0:128], maskT_d, [], [b_xin[1]], "d_xin1")
        for g in range(8):
            TT("dve", wspT[:, g, :], stg[:, g, :], xin[1][:, 0:128], ALU.mult, [b_xin[0], b_xin[1]], [b_const])
        ACT(cch[:], cvec[:, 56:64], AF.Exp, [b_const], [b_const], scale=-1.0)
        ACT(cch[:], cch[:], AF.Ln, [b_const], [b_const], bias=1.0)
        TS("dve", cch[:], cch[:], -8.0, None, ALU.mult, None, [b_const], [b_const])
        TS("dve", chf[:, 0:8], cch[:], 0.5, None, ALU.mult, None, [b_const], [b_const])
        TS("dve", chf[:, 8:24], cvec[:, 40:56], 0.5, None, ALU.mult, None, [b_const], [b_const])
        MEMSET("dve", cpow[:, 0:1], -0.5, [b_const])
        MEMSET("dve", cpow[:, 1:2], 0.5, [b_const])

        cw = lambda k, c: cvec[:, k * 8 + c:k * 8 + c + 1]
        cb_ = lambda c: cvec[:, 32 + c:33 + c]
        cba = lambda c: cvec[:, 40 + c:41 + c]
        cbi = lambda c: cvec[:, 48 + c:49 + c]

        def layer_norm_multi(items, g_ap, b_ap, b_gb, ss, eps=LN_EPS):
            n = len(items)
            B = ss["B"]
            st = ss["st"].rearrange("p (j a b) -> p j a b", j=2, a=2)
            mv = ss["mv"].rearrange("p (j c) -> p j c", j=2)
            for j, (src, b_src, dst, b_dst) in enumerate(items):
                for h in range(2):
                    S.op("dve", lambda e, j=j, h=h, src=src: e.bn_stats(out=st[:, j, h, :], in_=src[:, h * 512:(h + 1) * 512]),
                         reads=[b_src], writes=[B])
                S.op("dve", lambda e, j=j: e.bn_aggr(out=mv[:, j, :], in_=st[:, j, :, :].rearrange("p a b -> p (a b)")),
                     reads=[B], writes=[B])
            ACT(ss["rs"][:, 0:n], mv[:, 0:n, 1], AF.Sqrt, [B], [B], bias=float(eps), scale=1.0)
            S.op("dve", lambda e: e.reciprocal(out=ss["rs"][:, 0:n], in_=ss["rs"][:, 0:n]), reads=[B], writes=[B])
            for j, (src, b_src, dst, b_dst) in enumerate(items):
                STT(src, src, mv[:, j, 0:1], g_ap, ALU.subtract, ALU.mult, [b_src, B, b_gb], [b_src])
                STT(dst, src, ss["rs"][:, j:j + 1], b_ap, ALU.mult, ALU.add, [b_src, B, b_gb], [b_dst])

        def layer_norm(src, b_src, dst, b_dst, g_ap, b_ap, b_gb, ss, eps=LN_EPS):
            layer_norm_multi([(src, b_src, dst, b_dst)], g_ap, b_ap, b_gb, ss, eps)

        def kv_precompute(seq):
            for mt in range(2):
                DMA("sp", vg[mt], mem_d[seq * 256 + mt * 128: seq * 256 + (mt + 1) * 128, :], [], [b_vg[mt]], f"d_vg{mt}")
            for mt in range(2):
                for hf in range(2):
                    bk = nb()
                    for c in range(4):
                        cc = hf * 4 + c
                        TR(pb[bk][:, c * 128:(c + 1) * 128], vg[mt][:, cc * 128:(cc + 1) * 128], [b_vg[mt]], bk)
                    CP("act" if hf else "dve", memT[:, hf * 4:(hf + 1) * 4, mt * 128:(mt + 1) * 128],
                       pb[bk][:].rearrange("p (a b) -> p a b", a=4), [Pb[bk]], b_qT)
            for jb in range(2):
                si = wload("kv", jb)
                for j4 in range(4):
                    jc = jb * 4 + j4
                    bk = nb()
                    for k in range(8):
                        MM(pb[bk][:, 0:256], wslv[si][:, k, j4 * 128:(j4 + 1) * 128], memT[:, k, :], k == 0, k == 7,
                           [b_wsl[si]] + b_qT, bk)
                    CP("act" if j4 % 2 else "dve", kT[:, jc, :], pb[bk][:, 0:256], [Pb[bk]], [b_kT])
            for hb in range(2):
                si = wload("kv", 2 + hb)
                for mt in range(2):
                    bk = nb()
                    for k in range(8):
                        MM(pb[bk][:], memT[:, k, mt * 128:(mt + 1) * 128], wslv[si][:, k, :], k == 0, k == 7,
                           [b_wsl[si]] + b_qT, bk)
                    CP("act" if mt else "dve", vv[:, mt, hb * 512:(hb + 1) * 512], pb[bk][:], [Pb[bk]], [b_vv])

        def zfeat(si, j4, bk, par):
            for k in range(8):
                MM(pb[bk][:, 0:T], wslv[si][:, k, j4 * 128:(j4 + 1) * 128], h0Tp[par][:, k, :], k == 0, k == 7,
                   [b_wsl[si], b_h0Tp[par]], bk)

        def route(tile_idx):
            R = rt
            bk = nb("S")
            for k in range(8):
                MM(pb[bk][:, 0:64], h1Tf[:, k, :], wr[:, k, :], k == 0, k == 7, [b_h1Tf, b_const], bk)
            ACT(R["s"], pb[bk][:, 0:64], AF.Tanh, [Pb[bk]], [b_rt["s"]], scale=0.5)
            TS("dve", R["s"], R["s"], 0.5, 0.5, ALU.mult, ALU.add, [b_rt["s"]], [b_rt["s"]])
            TT("dve", R["ch"], R["s"], rbias[:], ALU.add, [b_rt["s"], b_const], [b_rt["ch"]])
            for g in range(8):
                S.op("dve", lambda e, g=g: e.max(out=R["o8"][:, g * 8:(g + 1) * 8], in_=R["ch"][:, g * 8:(g + 1) * 8]),
                     reads=[b_rt["ch"]], writes=[b_rt["o8"]])
            o8v = R["o8"].rearrange("p (g j) -> p g j", g=8)
            TT("dve", R["gs"], o8v[:, :, 0], o8v[:, :, 1], ALU.add, [b_rt["o8"]], [b_rt["gs"]])
            S.op("dve", lambda e: e.max(out=R["srt"], in_=R["gs"]), reads=[b_rt["gs"]], writes=[b_rt["srt"]])
            yield
            TS("dve", R["gm"], R["gs"], R["srt"][:, 3:4], None, ALU.is_ge, None, [b_rt["gs"], b_rt["srt"]], [b_rt["gm"]])
            TS("dve", R["nbg"], R["gm"], -1.0, 1e9, ALU.add, ALU.mult, [b_rt["gm"]], [b_rt["nbg"]])
            for g in range(8):
                TS("dve", R["msk"][:, g * 8:(g + 1) * 8], R["ch"][:, g * 8:(g + 1) * 8], R["gm"][:, g:g + 1],
                   R["nbg"][:, g:g + 1], ALU.mult, ALU.add, [b_rt["ch"], b_rt["gm"], b_rt["nbg"]], [b_rt["msk"]])
            yield
            S.op("dve", lambda e: e.max(out=R["t8"], in_=R["msk"]), reads=[b_rt["msk"]], writes=[b_rt["t8"]])
            TS("dve", R["sel"], R["msk"], R["t8"][:, 7:8], None, ALU.is_ge, None, [b_rt["msk"], b_rt["t8"]], [b_rt["sel"]])
            TT("dve", R["wun"], R["s"], R["sel"], ALU.mult, [b_rt["s"], b_rt["sel"]], [b_rt["wun"]])
            S.op("dve", lambda e: e.reduce_sum(out=R["den"][:, 0:1], in_=R["wun"], axis=AX.X),
                 reads=[b_rt["wun"]], writes=[b_rt["den"]])
            S.op("dve", lambda e: e.reciprocal(out=R["rd"][:, 0:1], in_=R["den"][:, 0:1]),
                 reads=[b_rt["den"]], writes=[b_rt["rd"]])
            TS("dve", wden[:, 0, :], R["wun"], R["rd"][:, 0:1], 2.5, ALU.mult, ALU.mult,
               [b_rt["wun"], b_rt["rd"]], [b_wden])
            yield
            CP("pool", selb, R["sel"], [b_rt["sel"]], [b_selb])
            bk = nb("S")
            MM(pb[bk][:, 0:64], ustr_bf[:], selb, True, True, [b_const, b_selb], bk)
            MM(pb[bk][:, 64:128], ones_bf[:], selb, True, True, [b_const, b_selb], bk)
            TT("dve", R["cnt"], pb[bk][:, 0:64], carry[:], ALU.add, [Pb[bk], b_carry], [b_rt["cnt"]])
            TT("dve", carry[:], pb[bk][:, 64:128], carry[:], ALU.add, [Pb[bk], b_carry], [b_carry])
            TS("dve", R["valid"], R["cnt"], float(CAP), None, ALU.is_lt, None, [b_rt["cnt"]], [b_rt["valid"]])
            TT("dve", R["dall"], R["cnt"], iotaC[:], ALU.add, [b_rt["cnt"], b_const], [b_rt["dall"]])
            STT(R["dall"], R["dall"], dummy[:, 0:1], R["valid"], ALU.subtract, ALU.mult,
                [b_rt["dall"], b_rt["valid"], b_const], [b_rt["dall"]])
            TS("dve", R["dall"], R["dall"], dummy[:, 0:1], None, ALU.add, None, [b_rt["dall"], b_const], [b_rt["dall"]])
            yield
            TT("dve", R["wv"], wden[:, 0, :], R["valid"], ALU.mult, [b_wden, b_rt["valid"]], [b_rt["wv"]])
            S.op("dve", lambda e: e.tensor_tensor_scan(out=R["csum"], data0=ones64[:], data1=R["sel"], initial=0.0,
                                                       op0=ALU.mult, op1=ALU.add),
                 reads=[b_const, b_rt["sel"]], writes=[b_rt["csum"]])
            TT("dve", R["csel"], R["csum"], R["sel"], ALU.mult, [b_rt["csum"], b_rt["sel"]], [b_rt["csel"]])
            oh3 = R["oh"].rearrange("p (k e) -> p k e", k=8)
            pr3 = R["pr"].rearrange("p (k e) -> p k e", k=8)
            bc = lambda ap: ap.unsqueeze(1).broadcast_to([128, 8, 64])
            yield
            B_oh = [b_vg[0]]
            B_pr = [b_vg[0]]
            TT("dve", oh3, kvals[:].unsqueeze(2).broadcast_to([128, 8, 64]), bc(R["csel"]), ALU.is_equal,
               [b_const, b_rt["csel"]], B_oh)
            TT("dve", pr3, oh3, bc(R["dall"]), ALU.mult, B_oh + [b_rt["dall"]], B_pr)
            S.op("dve", lambda e: e.reduce_sum(out=R["dstf"], in_=pr3, axis=AX.X), reads=B_pr, writes=[b_rt["dstf"]])
            CP("dve", dst_all[:, tile_idx, :], R["dstf"], [b_rt["dstf"]], [b_dst])
            TT("dve", pr3, oh3, bc(R["wv"]), ALU.mult, B_oh + [b_rt["wv"]], B_pr)
            S.op("dve", lambda e: e.reduce_sum(out=wk_all[:, tile_idx, :], in_=pr3, axis=AX.X),
                 reads=B_pr, writes=[b_wk])

        b_r1s = [Buf(f"r1s{i}") for i in range(32)]
        b_h1Ts = [Buf(f"h1Ts{i}") for i in range(2)]
        per = -(-(NEXP + 1) // n_tiles)

        def xload(ti):
            for s in range(2):
                t0 = ti * T + s * 128
                DMA("sp", HX[ti % 2][s], x_d[t0:t0 + 128, :], [], [b_HX[ti % 2][s]], f"d_xin{ti % 2}{s}")

        def stage_A(ti):
            par = ti % 2
            h0, b_h0 = HX[par], b_HX[par]
            seq = (ti * T) // SEQ
            if (ti * T) % SEQ == 0:
                kv_precompute(seq)
                MEMSET("pool", halo[:], 0.0, [b_halo])
                MEMSET("pool", hstate[:], 0.0, [b_hst])
            layer_norm_multi([(h0[s], b_h0[s], h0[s], b_h0[s]) for s in range(2)], lnv[:, 0, :], lnv[:, 1, :], b_lnv, ss_in[0])
            yield
            for s in range(2):
                for hf in range(2):
                    bk = nb("S")
                    for c in range(4):
                        cc = hf * 4 + c
                        TR(pb[bk][:, c * 128:(c + 1) * 128], h0[s][:, cc * 128:(cc + 1) * 128], [b_h0[s]], bk)
                    CP("act" if hf else "dve", h0Tp[par][:, hf * 4:(hf + 1) * 4, s * 128:(s + 1) * 128],
                       pb[bk][:].rearrange("p (a b) -> p a b", a=4), [Pb[bk]], [b_h0Tp[par]])
                yield

        def stage_B(ti):
            par = ti % 2
            hT, bhT = h0Tp[par], b_h0Tp[par]
            for hb in range(2):
                si = wload("in", 2 + hb, 2 * hb)
                for s in range(2):
                    bk = nb("S")
                    for k in range(8):
                        MM(pb[bk][:], hT[:, k, s * 128:(s + 1) * 128], wslv[si][:, k, :], k == 0, k == 7,
                           [b_wsl[si], bhT], bk)
                    ACT(vg[s][:, hb * 512:(hb + 1) * 512], pb[bk][:], AF.Gelu_apprx_tanh, [Pb[bk]], [b_vg[s]])
                yield
            layer_norm_multi([(vg[s], b_vg[s], vn[:, s, :], b_vn[s]) for s in range(2)], lnv[:, 2, :], lnv[:, 3, :], b_lnv, ss_v[0])
            yield
            for jb in range(2):
                si = wload("in", jb, 2 * jb)
                for j4 in range(4):
                    g = jb * 4 + j4
                    bk = nb("S")
                    zfeat(si, j4, bk, par)
                    ACT(ub[g % 2], pb[bk][:, 0:T], AF.Gelu_apprx_tanh, [Pb[bk]], [b_ub[g % 2]])
                    bk2 = nb("S")
                    for s in range(2):
                        MM(pb[bk2][:, s * 128:(s + 1) * 128], ones_bf[:], bspf[:, g * 128:(g + 1) * 128],
                           True, False, [b_const], bk2)
                        MM(pb[bk2][:, s * 128:(s + 1) * 128], vn[:, s, g * 128:(g + 1) * 128], wspT[:, g, :],
                           False, True, [b_const, b_vn[s]], bk2)
                    TT("dve", ybr0p[par][:, g, :], ub[g % 2], pb[bk2][:, 0:T], ALU.mult, [b_ub[g % 2], Pb[bk2]], [b_ybr0p[par][g]])
                yield

        def stage_B2(ti):
            par = ti % 2
            for jb in range(2):
                si = wload("in", 6 + jb)
                for j4 in range(4):
                    g = jb * 4 + j4
                    bk = nb("S")
                    zfeat(si, j4, bk, par)
                    ACT(glb[g], pb[bk][:, 0:T], AF.Gelu_apprx_tanh, [Pb[bk]], [b_glb[g]])
                yield

        def stage_C(ti):
            hcc = lambda g: chf[:, g:g + 1]
            hba = lambda g: chf[:, 8 + g:9 + g]
            hbi = lambda g: chf[:, 16 + g:17 + g]
            for jb in range(2):
                si_x = wload("in", 4, 1) if jb == 0 else 1
                G4 = [jb * 4 + c for c in range(4)]
                bx = {}
                for c4, g in enumerate(G4):
                    bx[c4] = nb("C")
                    zfeat(si_x, c4, bx[c4], ti % 2)
                if jb == 0:
                    wload("in", 5, 1)
                yield
                for c4, g in enumerate(G4):
                    L = lru[c4]
                    LB = L["B"]
                    CP("pool", L["xl"][:, 0:3], halo[:, g, 0:3], [b_halo], [LB["xl"]])
                    CP("act", L["xl"][:, 3:3 + T], pb[bx[c4]][:, 0:T], [Pb[bx[c4]]], [LB["xl"]])
                    CP("pool", halo[:, g, 0:3], L["xl"][:, T:T + 3], [LB["xl"]], [b_halo])
                yield
                for c4, g in enumerate(G4):
                    L = lru[c4]
                    LB = L["B"]
                    ACT(L["xc"], L["xl"][:, 3:3 + T], AF.Identity, [LB["xl"], b_const], [LB["xc"]], bias=cb_(g), scale=cw(0, g))
                yield
                for k in range(1, 4):
                    for c4, g in enumerate(G4):
                        L = lru[c4]
                        LB = L["B"]
                        STT(L["xc"], L["xl"][:, 3 - k:3 - k + T], cw(k, g), L["xc"], ALU.mult, ALU.add,
                            [LB["xl"], LB["xc"], b_const], [LB["xc"]])
                    yield
                for c4, g in enumerate(G4):
                    L = lru[c4]
                    CP("act", xcb[c4], L["xc"], [L["B"]["xc"]], [b_xcb[c4]])
                yield
                b1 = {}
                for c4, g in enumerate(G4):
                    b1[c4] = nb("C")
                    MM(pb[b1[c4]][:, 0:T], wa[:, g, :], xcb[c4], True, True, [b_const, b_xcb[c4]], b1[c4])
                    MM(pb[b1[c4]][:, T:2 * T], wi[:, g, :], xcb[c4], True, True, [b_const, b_xcb[c4]], b1[c4])
                yield
                for c4, g in enumerate(G4):
                    L = lru[c4]
                    ACT(L["ra"], pb[b1[c4]][:, 0:T], AF.Tanh, [Pb[b1[c4]], b_const], [L["B"]["ra"]], bias=hba(g), scale=0.5)
                    ACT(L["ib"], pb[b1[c4]][:, T:2 * T], AF.Tanh, [Pb[b1[c4]], b_const], [L["B"]["ib"]], bias=hbi(g), scale=0.5)
                yield
                for c4, g in enumerate(G4):
                    L = lru[c4]
                    ACT(L["ra"], L["ra"], AF.Exp, [L["B"]["ra"], b_const], [L["B"]["ra"]], bias=hcc(g), scale=hcc(g))
                yield
                for c4, g in enumerate(G4):
                    L = lru[c4]
                    LB = L["B"]
                    STT(L["ib"], L["ib"], 1.0, L["xc"], ALU.add, ALU.mult, [LB["ib"], LB["xc"]], [LB["ib"]])
                    TT("pool", L["mh"], L["ra"], L["ra"], ALU.mult, [LB["ra"]], [LB["mh"]])
                yield
                mhB = [lru[c4]["B"]["mh"] for c4 in range(4)]
                ACT(mh4, mh4, AF.Sqrt, mhB, mhB, bias=0.25, scale=-0.25)
                yield
                for c4, g in enumerate(G4):
                    L = lru[c4]
                    LB = L["B"]
                    TT("pool", L["ib"], L["ib"], L["mh"], ALU.mult, [LB["ib"], LB["mh"]], [LB["ib"]])
                yield
                for c4, g in enumerate(G4):
                    L = lru[c4]
                    LB = L["B"]
                    S.op("dve", lambda e, L=L, g=g: e.tensor_tensor_scan(out=L["mh"], data0=L["ra"], data1=L["ib"],
                                                                         initial=hstate[:, g:g + 1], op0=ALU.mult, op1=ALU.add),
                         reads=[LB["ra"], LB["ib"], b_hst], writes=[LB["mh"]])
                yield
                for c4, g in enumerate(G4):
                    L = lru[c4]
                    LB = L["B"]
                    CP("pool", hstate[:, g:g + 1], L["mh"][:, T - 1:T], [LB["mh"]], [b_hst])
                    TT("pool", ybr[1][:, g, :], L["mh"], glb[g], ALU.mult, [LB["mh"], b_glb[g]], [b_ybr[1][g]])
                yield

        def stage_D(ti):
            for jb in range(2):
                si = wload("in", 8, 3) if jb == 0 else 3
                for j4 in range(4):
                    j = jb * 4 + j4
                    bk = nb("D")
                    zfeat(si, j4, bk, ti % 2)
                    CP("act", qT[:, j, :], pb[bk][:, 0:T], [Pb[bk]], [b_qT[j]])
                    if j4 == 3 and jb == 0:
                        wload("in", 9, 3)
                    yield
            for hh in range(4):
                bS = nb("D")
                for mc in range(2):
                    for dc in range(2):
                        MM(pb[bS][:, mc * T:(mc + 1) * T], kT[:, 2 * hh + dc, mc * 128:(mc + 1) * 128], qT[:, 2 * hh + dc, :],
                           dc == 0, dc == 1, [b_kT, b_qT[2 * hh + dc]], bS)
                ex = expT[hh % 2]
                yield
                ACT(ex, pb[bS][:].rearrange("p (m t) -> p m t", m=2), AF.Exp, [Pb[bS]], [b_expT[hh % 2]], scale=1.0 / 16.0)
                bD = nb("D")
                for mc in range(2):
                    MM(pb[bD][:, 0:T], ones_bf[:], ex[:, mc, :], mc == 0, mc == 1, [b_const, b_expT[hh % 2]], bD)
                bO = nb("D")
                for dc in range(2):
                    for mc in range(2):
                        MM(pb[bO][:, dc * T:(dc + 1) * T], vv[:, mc, hh * 256 + dc * 128: hh * 256 + (dc + 1) * 128],
                           ex[:, mc, :], mc == 0, mc == 1, [b_vv, b_expT[hh % 2]], bO)
                yield
                S.op("dve", lambda e, bD=bD: e.reciprocal(out=rden, in_=pb[bD][:, 0:T]), reads=[Pb[bD]], writes=[b_rden])
                for dc in range(2):
                    TT("dve", ybr[2][:, 2 * hh + dc, :], pb[bO][:, dc * T:(dc + 1) * T], rden, ALU.mult,
                       [Pb[bO], b_rden], [b_ybr[2][2 * hh + dc]])

        def stage_E(ti):
            pc = 0
            for jb in range(2):
                for k in range(3):
                    sgt = wload("in", 10 + 2 * k + jb)
                    sbr = wload(f"br{k}", jb)
                    for j4 in range(4):
                        j = jb * 4 + j4
                        bk = nb()
                        zfeat(sgt, j4, bk, ti % 2)
                        sg_i = pc % 2
                        pc += 1
                        ACT(sig[sg_i], pb[bk][:, 0:T], AF.Tanh, [Pb[bk]], [b_sig[sg_i]], scale=0.5)
                        bkp = nb()
                        for c in range(8):
                            yk = ybr0p[ti % 2] if k == 0 else ybr[k]
                            byk = b_ybr0p[ti % 2] if k == 0 else b_ybr[k]
                            MM(pb[bkp][:, 0:T], wslv[sbr][:, c, j4 * 128:(j4 + 1) * 128], yk[:, c, :], c == 0, c == 7,
                               [b_wsl[sbr], byk[c]], bkp)
                        if k == 0:
                            STT(macc[j4], sig[sg_i], 1.0, pb[bkp][:, 0:T], ALU.add, ALU.mult, [b_sig[sg_i], Pb[bkp]], [b_macc[j4]])
                        else:
                            STT(prod[sg_i], sig[sg_i], 1.0, pb[bkp][:, 0:T], ALU.add, ALU.mult, [b_sig[sg_i], Pb[bkp]], [b_prod[sg_i]])
                            if k == 1:
                                TT("pool", macc[j4], macc[j4], prod[sg_i], ALU.add, [b_macc[j4], b_prod[sg_i]], [b_macc[j4]])
                            else:
                                TT("pool", mrg[:, j, :], macc[j4], prod[sg_i], ALU.add, [b_macc[j4], b_prod[sg_i]], [b_mrg[j]])
                    yield

        def stage_F(ti):
            so = [wload("out", 0), wload("out", 1)]
            h0, b_h0 = HX[ti % 2], b_HX[ti % 2]
            for s in range(2):
                for hb in range(2):
                    bk = nb()
                    for k in range(8):
                        MM(pb[bk][:], mrg[:, k, s * 128:(s + 1) * 128], wslv[so[hb]][:, k, :], k == 0, k == 7,
                           [b_wsl[so[hb]], b_mrg[k]], bk)
                    STT(r1[s][:, hb * 512:(hb + 1) * 512], h0[s][:, hb * 512:(hb + 1) * 512], 2.0 * ALPHA, pb[bk][:], ALU.mult, ALU.add,
                        [b_h0[s], Pb[bk]], [b_r1[s]])
            if debug and ti == 0:
                for k in range(3):
                    yk = ybr0p[ti % 2] if k == 0 else ybr[k]
                    byk = b_ybr0p[ti % 2] if k == 0 else b_ybr[k]
                    DMA("sp", dbg_y[k].rearrange("p (k t) -> p k t", k=8), yk, [byk[c] for c in range(8)], [], "d_dbg")
                DMA("sp", dbg_y[3].rearrange("p (k t) -> p k t", k=8), mrg, b_mrg, [], "d_dbg")

        def stage_G(ti, s):
            t0 = ti * T + s * 128
            layer_norm(r1[s], b_r1[s], h1t, b_h1t, lnv[:, 4, :], lnv[:, 5, :], b_lnv, ss_1, eps=4.0 * LN_EPS)
            yield
            ACT(r1[s], h1t, AF.Copy, [b_h1t], [b_r1[s]], scale=ALPHA)
            DMA("act", r1s[t0:t0 + 128, :], r1[s], [b_r1[s]], [b_r1s[ti * 2 + s]], f"d_r1{s}")
            for hf in range(2):
                bk = nb("S")
                for c in range(4):
                    cc = hf * 4 + c
                    TR(pb[bk][:, c * 128:(c + 1) * 128], h1t[:, cc * 128:(cc + 1) * 128], [b_h1t], bk)
                CP("act", h1Tf[:, hf * 4:(hf + 1) * 4, :], pb[bk][:].rearrange("p (a b) -> p a b", a=4), [Pb[bk]], [b_h1Tf])
            yield
            CP("act", h1Tb[:, :, s * 128:(s + 1) * 128], h1Tf, [b_h1Tf], [b_h1Tb])
            yield from route(ti * 2 + s)
            hp = (ti * 2 + s) % 2
            CP("act", h1b[hp], h1t, [b_h1t], [b_h1b[hp]])
            for k in range(8):
                tix = ti * 2 + s
                S.op("pool", lambda e, hp=hp, tix=tix, k=k: e.indirect_dma_start(
                    out=Xs, out_offset=bass.IndirectOffsetOnAxis(ap=dst_all[:, tix, k:k + 1], axis=0),
                    in_=h1b[hp], in_offset=None),
                    reads=[b_h1b[hp], b_dst], writes=[], dma=f"d_sc{hp}")
            if s == 1:
                seq_ = (ti * T) // SEQ
                tpos = (ti * T) % SEQ
                DMA("act", h1Ts[seq_].rearrange("p (k t) -> p k t", k=8)[:, :, tpos:tpos + T], h1Tb, [b_h1Tb], [b_h1Ts[seq_]], "d_h1T")

        def run_interleaved(gens, until=None, weights=None):
            gens = list(gens)
            wts = {id(g_): (weights[i] if weights else 1) for i, g_ in enumerate(gens)}
            while gens:
                if until is not None and not any(g_ in gens for g_ in until):
                    return gens
                for g_ in list(gens):
                    for _ in range(wts[id(g_)]):
                        try:
                            next(g_)
                        except StopIteration:
                            gens.remove(g_)
                            break
            return []

        def G_both(ti):
            yield from stage_G(ti, 0)
            yield from stage_G(ti, 1)

        def AB(ti):
            yield from stage_A(ti)
            yield from stage_B(ti)

        def B2G(ti_next, ti_prev):
            if ti_next < n_tiles:
                yield from stage_B2(ti_next)
            if ti_prev >= 0:
                yield from G_both(ti_prev)

        xload(0)
        if n_tiles > 1:
            xload(1)
        run_interleaved([AB(0)])
        run_interleaved([stage_B2(0)])
        for ti in range(n_tiles):
            nxt = ti + 1
            seq_start_next = nxt < n_tiles and (nxt * T) % SEQ == 0
            gens = [stage_C(ti), stage_D(ti)]
            if nxt < n_tiles and not seq_start_next:
                gens.append(AB(nxt))
            run_interleaved(gens)
            if seq_start_next:
                run_interleaved([AB(nxt)])
            run_interleaved([stage_E(ti), B2G(ti + 1, ti - 1)], weights=[1, 3])
            stage_F(ti)
            if ti + 2 < n_tiles:
                xload(ti + 2)
        run_interleaved([G_both(n_tiles - 1)])

        if debug:
            DMA("sp", dbg_wden, wk_all[:], [b_wk], [], "d_dbg")
            DMA("sp", dbg_dst, dst_all[:], [b_dst], [], "d_dbg")
        S.barrier()
        foff[0] = 0
        boff[0] = 0
        NST = CAP // 128
        xgT = [br(8 * CAP).rearrange("p (k t) -> p k t", k=8) for _ in range(1)]
        b_xgT = [Buf(f"xgT{i}") for i in range(1)]
        NXS = 8
        xs = [br(1024) for _ in range(NXS)]
        b_xs = [Buf(f"xs{i}") for i in range(NXS)]
        sgx = br(2 * 512).rearrange("p (f t) -> p f t", f=2)
        b_sgx = Buf("sgx")
        acx = [br(2 * CAP).rearrange("p (f t) -> p f t", f=2) for _ in range(1)]
        b_acx = [Buf(f"acx{i}") for i in range(1)]
        NYT = 8
        yt = [br(1024) for _ in range(NYT)]
        b_yt = [Buf(f"yt{i}") for i in range(NYT)]
        xs_ctr = [0]
        yt_ctr = [0]
        n_e = NEXP if n_exp > 2 else 2

        wst = [fr(6144) for _ in range(2)]
        b_wst = [Buf(f"wst{i}") for i in range(2)]
        wst_ctr = [0]

        def WL(e, a=None, stage=None):
            if a is None:
                a = 2 * (e % 2)
            if e < NEXP:
                g_src, u_src, d_src = eg_d[e], eu_d[e], ed_d[e]
            else:
                g_src, u_src, d_src = sg_d, su_d, sd_d
            if stage is None:
                wi_ = wst_ctr[0] % 2
                wst_ctr[0] += 1
                W, bW, key = wst[wi_], b_wst[wi_], f"d_wst{wi_}"
            else:
                W, bW, key = stage
            gv = W[:, 0:2048].rearrange("p (k f) -> p k f", k=8)
            uv = W[:, 2048:4096].rearrange("p (k f) -> p k f", k=8)
            dv = W[:, 4096:6144]
            DMA("sp", gv, g_src.rearrange("(k p) f -> p k f", p=128), [], [bW], key)
            DMA("sp", uv, u_src.rearrange("(k p) f -> p k f", p=128), [], [], key)
            DMA("sp", dv.rearrange("p (f d) -> p f d", f=2), d_src.rearrange("(f p) d -> p f d", p=128), [], [],
                key)
            bW.w = (key, S.dma_cnt[key])
            CP("dve", wslv[a][:, :, 0:256], gv, [bW], [b_wsl[a]])
            CP("act", wslv[a][:, :, 256:512], uv, [bW], [b_wsl[a]])
            CP("pool", wsl[:, a + 1, 0:2048], dv, [bW], [b_wsl[a + 1]])

        def TX(e):
            X = xgT[0]
            for st in range(NST):
                xi = xs_ctr[0] % NXS
                xs_ctr[0] += 1
                r0 = e * CAP + st * 128
                DMA("sp", xs[xi], Xs[r0:r0 + 128, :], [], [b_xs[xi]], f"d_xs{xi}")
                bk = nb()
                pbv = pb[bk][:].bitcast(BF16)
                for k in range(8):
                    S.op("pe", lambda e_, xi=xi, k=k, pbv=pbv: e_.transpose(out=pbv[:, k * 128:(k + 1) * 128],
                                                                             in_=xs[xi][:, k * 128:(k + 1) * 128],
                                                                             identity=ident_bf[:]),
                         reads=[b_xs[xi], b_const], writes=[Pb[bk]])
                CP("act" if st % 2 else "dve", X[:, :, st * 128:(st + 1) * 128],
                   pbv.rearrange("p (k t) -> p k t", k=8), [Pb[bk]], [b_xgT[0]])

        def GUX(e):
            a = 2 * (e % 2)
            X = xgT[0]
            groups = [(c0, min(512, CAP - c0)) for c0 in range(0, CAP, 512)]
            for (c0, cn) in groups:
                banks = [nb() for _ in range(4)]
                for fc in range(2):
                    for gu in range(2):
                        bk = banks[2 * gu + fc]
                        for k in range(8):
                            MM(pb[bk][:, 0:cn], wslv[a][:, k, gu * 256 + fc * 128: gu * 256 + (fc + 1) * 128],
                               X[:, k, c0:c0 + cn], k == 0, k == 7, [b_wsl[a], b_xgT[0]], bk)
                for fc in range(2):
                    ACT(sgx[:, fc, 0:cn], pb[banks[fc]][:, 0:cn], AF.Silu, [Pb[banks[fc]]], [b_sgx])
                    TT("dve", acx[0][:, fc, c0:c0 + cn], sgx[:, fc, 0:cn], pb[banks[2 + fc]][:, 0:cn], ALU.mult,
                       [b_sgx, Pb[banks[2 + fc]]], [b_acx[0]])

        def DNX(e):
            a = 2 * (e % 2)
            dn = wsl[:, a + 1, 0:2048].rearrange("p (f d) -> p f d", f=2)
            for st in range(NST):
                yi = yt_ctr[0] % NYT
                yt_ctr[0] += 1
                for hb in range(2):
                    bk = nb()
                    for fc in range(2):
                        MM(pb[bk][:], acx[0][:, fc, st * 128:(st + 1) * 128], dn[:, fc, hb * 512:(hb + 1) * 512],
                           fc == 0, fc == 1, [b_acx[0], b_wsl[a + 1]], bk)
                    CP("act" if hb else "dve", yt[yi][:, hb * 512:(hb + 1) * 512], pb[bk][:], [Pb[bk]], [b_yt[yi]])
                r0 = e * CAP + st * 128
                DMA("pool", Ys[r0:r0 + 128, :], yt[yi], [b_yt[yi]], [], f"d_yt{yi}")

        WL(0)
        WL(1)
        TX(0)
        for e in range(n_e):
            GUX(e)
            if e + 1 < n_e:
                TX(e + 1)
            DNX(e)
            if e + 2 < n_e:
                WL(e + 2)

        S.barrier()
        foff[0] = 0
        boff[0] = 0
        acc = [fr(1024) for _ in range(16)]
        b_acc = [Buf(f"acc{i}") for i in range(16)]
        ss_m = dict(st=fr(24), mv=fr(4), rs=fr(2), B=Buf("ss_m"))
        wst_c = fr(6144)
        b_wst_c = Buf("wst_c")
        ln2v = fr(2048).rearrange("p (a d) -> p a d", a=2)
        DMA("sp", ln2v, lnvec_d[:, 6:8, :], [], [b_ln2v], "d_c2")
        h1T = br(8 * SEQ).rearrange("p (k t) -> p k t", k=8)
        b_h1T = Buf("h1T")
        sgb = [br(1024).rearrange("p (f t) -> p f t", f=2) for _ in range(2)]
        b_sgb = [Buf(f"sgb{i}") for i in range(2)]
        actT = [br(1024).rearrange("p (f t) -> p f t", f=2) for _ in range(2)]
        b_actT = [Buf(f"actT{i}") for i in range(2)]
        gbuf = [br(1024) for _ in range(6)]
        b_gbuf = [Buf(f"gbuf{i}") for i in range(6)]
        g_ctr = [0]
        dg = [br(128) for _ in range(4)]
        b_dg = [Buf(f"dg{i}") for i in range(4)]
        dg_ctr = [0]
        wsp = [dict(bf=br(8), hi=fr(8), lo=fr(8), B=Buf(f"wsp{i}")) for i in range(2)]

        n_seq = max(1, (n_tiles * T) // SEQ)
        for seq in range(n_seq):
            DMA("sp", h1T.rearrange("p k t -> p (k t)"), h1Ts[seq], [b_h1Ts[seq]], [b_h1T], "d_h1Tl")
            for i in range(16):
                t0 = seq * SEQ + i * 128
                DMA("sp", acc[i], r1s[t0:t0 + 128, :], [b_r1s[seq * 16 + i]], [b_acc[i]], f"d_acc{i}")
            steps = [(NEXP, tt) for tt in range(4)]
            a = 0
            if seq == 0:
                WL(NEXP, 0, (wst_c, b_wst_c, "d_wstc"))

            def GU(idx):
                e, tt = steps[idx]
                grp = idx % 2
                par = idx % 2
                for fc in range(2):
                    for gu in range(2):
                        bk = 4 * grp + 2 * gu + fc
                        for k in range(8):
                            MM(pb[bk][:], wslv[a][:, k, gu * 256 + fc * 128: gu * 256 + (fc + 1) * 128],
                               h1T[:, k, tt * 512:(tt + 1) * 512], k == 0, k == 7, [b_wsl[a], b_h1T], bk)
                for fc in range(2):
                    ACT(sgb[par][:, fc, :], pb[4 * grp + fc][:], AF.Silu, [Pb[4 * grp + fc]], [b_sgb[par]])
                    TT("dve", actT[par][:, fc, :], sgb[par][:, fc, :], pb[4 * grp + 2 + fc][:], ALU.mult,
                       [b_sgb[par], Pb[4 * grp + 2 + fc]], [b_actT[par]])

            def DOWN(idx):
                e, tt = steps[idx]
                grp = idx % 2
                par = idx % 2
                dn = wsl[:, a + 1, 0:2048].rearrange("p (f d) -> p f d", f=2)
                for st in range(4):
                    tile_i = tt * 4 + st
                    for hb in range(2):
                        bk = 4 * grp + 2 * (st % 2) + hb
                        for fc in range(2):
                            MM(pb[bk][:], actT[par][:, fc, st * 128:(st + 1) * 128], dn[:, fc, hb * 512:(hb + 1) * 512],
                               fc == 0, fc == 1, [b_actT[par], b_wsl[a + 1]], bk)
                        TT("dve", acc[tile_i][:, hb * 512:(hb + 1) * 512], pb[bk][:], acc[tile_i][:, hb * 512:(hb + 1) * 512],
                           ALU.add, [Pb[bk], b_acc[tile_i]], [b_acc[tile_i]])

            GU(0)
            for idx in range(len(steps)):
                if idx + 1 < len(steps):
                    GU(idx + 1)
                DOWN(idx)
            cbanks = {}

            def CG(i):
                tix = seq * 16 + i
                W = wsp[i % 2]
                CP("dve", W["bf"], wk_all[:, tix, :], [b_wk], [W["B"]])
                CP("dve", W["hi"], W["bf"], [W["B"]], [W["B"]])
                TT("dve", W["lo"], wk_all[:, tix, :], W["hi"], ALU.subtract, [b_wk, W["B"]], [W["B"]])
                bks = [nb(), nb()]
                cbanks[i] = bks
                for k in range(8):
                    gi = g_ctr[0] % 6
                    g_ctr[0] += 1
                    S.op("pool", lambda e_, gi=gi, tix=tix, k=k: e_.indirect_dma_start(
                        out=gbuf[gi], out_offset=None, in_=Ys,
                        in_offset=bass.IndirectOffsetOnAxis(ap=dst_all[:, tix, k:k + 1], axis=0)),
                        reads=[b_dst], writes=[b_gbuf[gi]], dma=f"d_g{gi}")
                    for part, nm in enumerate(("hi", "lo")):
                        di = dg_ctr[0] % 4
                        dg_ctr[0] += 1
                        TS("dve", dg[di], ident_bf[:], W[nm][:, k:k + 1], None, ALU.mult, None, [b_const, W["B"]], [b_dg[di]])
                        for hb in range(2):
                            MM(pb[bks[hb]][:], dg[di], gbuf[gi][:, hb * 512:(hb + 1) * 512], k == 0 and part == 0,
                               k == 7 and part == 1, [b_dg[di], b_gbuf[gi]], bks[hb])

            def CL(i):
                t0 = seq * SEQ + i * 128
                bks = cbanks[i]
                for hb in range(2):
                    TT("dve", acc[i][:, hb * 512:(hb + 1) * 512], pb[bks[hb]][:], acc[i][:, hb * 512:(hb + 1) * 512],
                       ALU.add, [Pb[bks[hb]], b_acc[i]], [b_acc[i]])
                layer_norm(acc[i], b_acc[i], acc[i], b_acc[i], ln2v[:, 0, :], ln2v[:, 1, :], b_ln2v, ss_m)
                DMA("act", out_d[t0:t0 + 128, :], acc[i], [b_acc[i]], [], f"d_out{i}")

            valid = [i for i in range(16) if seq * 16 + i < n_tiles * 2]
            for j, i in enumerate(valid):
                if j == 0:
                    CG(i)
                if j + 1 < len(valid):
                    CG(valid[j + 1])
                CL(i)

        sems = {k: es.enter_context(nc.semaphore(k)) for k in S.sem_keys()}
        finals = [(k, v) for k, v in S.dma_cnt.items() if k.startswith("d_out")]
        S.emit(nc, sems, finals)
    return nc, S


def _prep_shared(inp):
    f = np.float32
    c = lambda a: np.ascontiguousarray(a, dtype=f)
    lnv = np.stack([inp["ln_in_g"], inp["ln_in_b"], inp["gmlp_norm_g"][0], inp["gmlp_norm_b"][0],
                    inp["ln1_g"][0], inp["ln1_b"][0], inp["ln2_g"][0], inp["ln2_b"][0]], axis=0)
    lnvec = c(np.broadcast_to(lnv[None], (128, 8, D)))
    col = lambda v: np.asarray(v).reshape(8, 128).T
    convw = np.asarray(inp["conv_w"][0]).reshape(4, 8, 128).transpose(2, 0, 1).reshape(128, 32)
    cvec = c(np.concatenate([convw, col(inp["conv_b"][0]), col(inp["lru_ba"][0]), col(inp["lru_bi"][0]),
                             col(inp["lru_lambda"][0])], axis=1))
    wspT = c(np.asarray(inp["gmlp_spatial_w"][0]).transpose(2, 0, 1))
    maskT = c(np.triu(np.ones((128, 128), f)))
    shared = {
        "lnvec": lnvec, "cvec": cvec, "wspT": wspT, "maskT": maskT,
        "bsp": c(np.asarray(inp["gmlp_spatial_b"][0]).reshape(1, 1024)),
        "wa": c(np.asarray(inp["lru_wa"][0]).transpose(1, 0, 2)),
        "wi": c(np.asarray(inp["lru_wi"][0]).transpose(1, 0, 2)),
        "wr": c(np.asarray(inp["w_router"][0]).reshape(8, 128, 64).transpose(1, 0, 2)),
        "rbias": c(np.broadcast_to(np.asarray(inp["router_bias"][0])[None], (128, 64))),
        "ident": c(np.eye(128)),
        "iotaC": c(np.broadcast_to((np.arange(64, dtype=f) * CAP)[None], (128, 64))),
        "ustrict": c(np.triu(np.ones((128, 128), f), 1)),
        "dummyidx": c((NSLOT + np.arange(128, dtype=f)).reshape(128, 1)),
        "kvals": c(np.broadcast_to(np.arange(1, 9, dtype=f)[None], (128, 8))),
        "w_in": c(inp["w_in"][0]), "w_kv": c(inp["w_kv"][0]),
        "w_br0": c(inp["w_br_gmlp"][0]), "w_br1": c(inp["w_br_lru"][0]), "w_br2": c(inp["w_br_xa"][0]),
        "w_out": c(inp["w_out"][0]),
        "exp_gate": c(inp["exp_gate"][0]), "exp_up": c(inp["exp_up"][0]), "exp_down": c(inp["exp_down"][0]),
        "sh_gate": c(inp["sh_gate"][0]), "sh_up": c(inp["sh_up"][0]), "sh_down": c(inp["sh_down"][0]),
    }
    return shared


def kernel(**inputs):
    inp = {k: np.asarray(v) for k, v in inputs.items()}
    shared = _prep_shared(inp)
    x = np.ascontiguousarray(inp["x"], dtype=np.float32)
    mem = np.ascontiguousarray(inp["mem"], dtype=np.float32)
    nc, _ = build()
    in_maps = []
    for c in range(NCORES):
        m = dict(shared)
        m["x"] = x[2 * c:2 * c + 2].reshape(TOK, D)
        m["mem"] = mem[2 * c:2 * c + 2].reshape(512, D)
        in_maps.append(m)
    res = run_bass_kernel_spmd(nc, in_maps, core_ids=list(range(NCORES)))
    out = np.concatenate([np.asarray(r["out"]).reshape(2, SEQ, D) for r in res.results], axis=0)
    return out.astype(np.float32)
```

```python
import bisect
from contextlib import ExitStack

import numpy as np
import concourse.bass as bass
import concourse.mybir as mybir
from concourse.bass_utils import run_bass_kernel_spmd

F32 = mybir.dt.float32
BF16 = mybir.dt.bfloat16
AF = mybir.ActivationFunctionType
ALU = mybir.AluOpType
AX = mybir.AxisListType

COMPUTE = ("pe", "act", "dve", "pool")
ENGS = ("pe", "act", "dve", "pool", "sp")

NCORES = 8
D = 1024
SEQ = 2048
TOK = 2 * SEQ
T = 256
NT = TOK // T
LN_EPS = 1e-5
ALPHA = 2.0 ** 0.25
NEXP = 64
CAP = 896
NSLOT = NEXP * CAP


class Buf:
    __slots__ = ("name", "w", "r", "excl")

    def __init__(self, name, excl=False):
        self.name = name
        self.w = None
        self.r = {}
        self.excl = excl


class Sched:
    def __init__(self):
        self.ops = {e: [] for e in ENGS}
        self.cnt = {e: 0 for e in COMPUTE}
        self.waited = {e: {} for e in ENGS}
        self.dma_cnt = {}
        self.pending = {e: [] for e in ENGS}

    def barrier(self):
        evs = [(e, self.cnt[e]) for e in COMPUTE if self.cnt[e] > 0]
        evs += [(k, v) for k, v in self.dma_cnt.items()]
        for e in ENGS:
            self.pending[e] = list(evs)

    def op(self, eng, fn, reads=(), writes=(), dma=None):
        waits = []
        wd = self.waited[eng]

        def need(ev, raw):
            if ev is None:
                return
            key, val = ev
            if key == eng and dma is None:
                if eng == "pe":
                    return
            if wd.get(key, 0) >= val:
                return
            wd[key] = val
            waits.append((key, val))

        if self.pending[eng]:
            for ev in self.pending[eng]:
                need(ev, True)
            self.pending[eng] = []
        for b in reads:
            need(b.w, True)
            if b.excl:
                for k, v in b.r.items():
                    need((k, v), False)
        for b in writes:
            need(b.w, False)
            for k, v in b.r.items():
                need((k, v), False)
        if dma is None:
            self.cnt[eng] += 1
            ev = (eng, self.cnt[eng])
        else:
            self.dma_cnt[dma] = self.dma_cnt.get(dma, 0) + 16
            ev = (dma, self.dma_cnt[dma])
        for b in writes:
            b.w = ev
            b.r = {}
        for b in reads:
            if b.r.get(ev[0], 0) < ev[1]:
                b.r[ev[0]] = ev[1]
        self.ops[eng].append((waits, fn, ev))
        return ev

    def sem_keys(self):
        return list(COMPUTE) + sorted(self.dma_cnt.keys())

    def emit(self, nc, sems, final_waits=()):
        sig = {e: set() for e in COMPUTE}
        for eng in ENGS:
            for waits, fn, ev in self.ops[eng]:
                for k, v in waits:
                    if k in sig:
                        sig[k].add(v)
        sigl = {e: sorted(sig[e]) for e in COMPUTE}

        def val(k, v):
            if k in sigl:
                return bisect.bisect_left(sigl[k], v) + 1
            return v

        with nc.Block() as block:
            def run(eng_name, e):
                for waits, fn, ev in self.ops[eng_name]:
                    for k, v in waits:
                        e.wait_ge(sems[k], val(k, v))
                    ins = fn(e)
                    if ev[0] in sig:
                        if ev[1] in sig[ev[0]]:
                            ins.then_inc(sems[ev[0]], 1)
                    else:
                        ins.then_inc(sems[ev[0]], 16)
                if eng_name == "sp":
                    for k, v in final_waits:
                        e.wait_ge(sems[k], v)

            @block.tensor
            def _(e):
                run("pe", e)

            @block.scalar
            def _(e):
                run("act", e)

            @block.vector
            def _(e):
                run("dve", e)

            @block.gpsimd
            def _(e):
                run("pool", e)

            @block.sync
            def _(e):
                run("sp", e)


def build(n_tiles=NT, n_exp=NEXP + 1, debug=False):
    nc = bass.Bass("TRN2", target_bir_lowering=False)
    S = Sched()

    def din(name, shape, dt=F32):
        return nc.dram_tensor(name, shape, dt, kind="ExternalInput").ap()

    x_d = din("x", [TOK, D])
    mem_d = din("mem", [512, D])
    lnvec_d = din("lnvec", [128, 8, D])
    cvec_d = din("cvec", [128, 64])
    wspT_d = din("wspT", [128, 8, 128])
    maskT_d = din("maskT", [128, 128])
    bsp_d = din("bsp", [1, 1024])
    wa_d = din("wa", [128, 8, 128])
    wi_d = din("wi", [128, 8, 128])
    wr_d = din("wr", [128, 8, 64])
    rbias_d = din("rbias", [128, 64])
    ident_d = din("ident", [128, 128])
    iotaC_d = din("iotaC", [128, 64])
    ustrict_d = din("ustrict", [128, 128])
    dummy_d = din("dummyidx", [128, 1])
    kvals_d = din("kvals", [128, 8])
    w_in_d = din("w_in", [D, 8192])
    w_kv_d = din("w_kv", [D, 2048])
    w_br_d = [din(f"w_br{k}", [D, D]) for k in range(3)]
    w_out_d = din("w_out", [D, D])
    eg_d = din("exp_gate", [NEXP, D, 256])
    eu_d = din("exp_up", [NEXP, D, 256])
    ed_d = din("exp_down", [NEXP, 256, D])
    sg_d = din("sh_gate", [D, 256])
    su_d = din("sh_up", [D, 256])
    sd_d = din("sh_down", [256, D])
    out_d = nc.dram_tensor("out", [TOK, D], F32, kind="ExternalOutput").ap()
    wsc = nc.dram_tensor("wsc", [28, 128, 4096], BF16).ap()
    dk = dict(kind="ExternalOutput") if debug else {}
    r1s = nc.dram_tensor("r1s", [TOK, D], F32, **dk).ap()
    h1Ts = nc.dram_tensor("h1Ts", [2, 128, 8 * SEQ], BF16, **dk).ap()
    Xs = nc.dram_tensor("Xs", [NSLOT + 128, D], BF16).ap()
    Ys = nc.dram_tensor("Ys", [NSLOT + 128, D], BF16).ap()
    ews = nc.dram_tensor("ews", [NEXP + 1, 128, 6144], BF16).ap()
    if debug:
        dbg_wden = nc.dram_tensor("dbg_wden", [128, 32, 8], F32, kind="ExternalOutput").ap()
        dbg_dst = nc.dram_tensor("dbg_dst", [128, 32, 8], mybir.dt.uint32, kind="ExternalOutput").ap()
        dbg_y = nc.dram_tensor("dbg_y", [4, 128, 8 * T], BF16, kind="ExternalOutput").ap()

    es = ExitStack()
    with es:
        def sb(name, shape, dt):
            return es.enter_context(nc.sbuf_tensor("s_" + name, shape, dt))

        FSZ = 24848
        BSZ = 31360
        Freg = sb("Freg", [128, FSZ], F32)
        Breg = sb("Breg", [128, BSZ], BF16)
        wsl = sb("wsl", [128, 4, 4096], BF16)
        wden = sb("wden", [128, 1, 64], F32)
        ident_bf = sb("ident_bf", [128, 128], BF16)
        ustr_bf = sb("ustr_bf", [128, 128], BF16)
        iotaC = sb("iotaC", [128, 64], F32)
        dummy = sb("dummy", [128, 1], F32)
        kvals = sb("kvals", [128, 8], F32)
        carry = sb("carry", [128, 64], F32)
        ones64 = sb("ones64", [128, 64], F32)
        dst_all = sb("dst_all", [128, 32, 8], mybir.dt.uint32)
        wk_all = sb("wk_all", [128, 32, 8], F32)
        ident = sb("ident", [128, 128], F32)
        ones_bf = sb("ones_bf", [128, 128], BF16)
        wspT = sb("wspT", [128, 8, 128], BF16)
        bspf = sb("bspf", [128, 1024], BF16)
        wa = sb("wa", [128, 8, 128], BF16)
        wi = sb("wi", [128, 8, 128], BF16)
        wr = sb("wr", [128, 8, 64], F32)
        rbias = sb("rbias", [128, 64], F32)
        cvec = sb("cvec", [128, 64], F32)
        cch = sb("cch", [128, 8], F32)
        chf = sb("chf", [128, 24], F32)
        cpow = sb("cpow", [128, 2], F32)

        halo = sb("halo", [128, 8, 4], F32)
        hstate = sb("hstate", [128, 8], F32)
        pb = [es.enter_context(nc.psum_tensor(f"pb{i}", [128, 512], F32)) for i in range(8)]
        Pb = [Buf(f"pb{i}", True) for i in range(8)]
        bank_ctr = [0]

        pools = {"C": [0, 1, 2, 3], "D": [4, 5], "S": [6, 7]}
        pool_ctr = {k: 0 for k in pools}
        cur_pool = [None]

        def nb(pool=None):
            pool = pool or cur_pool[0]
            if pool is None:
                i = bank_ctr[0] % 8
                bank_ctr[0] += 1
                return i
            lst = pools[pool]
            i = lst[pool_ctr[pool] % len(lst)]
            pool_ctr[pool] += 1
            return i

        def MM(out, lhsT, rhs, start, stop, reads, bank):
            S.op("pe", lambda e: e.matmul(out, lhsT=lhsT, rhs=rhs, start=start, stop=stop),
                 reads=reads, writes=[Pb[bank]])

        b_ident = Buf("ident")

        def TR(out, in_, reads, bank):
            S.op("pe", lambda e: e.transpose(out=out, in_=in_, identity=ident[:]),
                 reads=list(reads) + [b_ident], writes=[Pb[bank]])

        def ACT(out, in_, func, reads, writes, bias=None, scale=None):
            kw = {}
            if bias is not None:
                kw["bias"] = bias
            if scale is not None:
                kw["scale"] = scale
            S.op("act", lambda e: e.activation(out=out, in_=in_, func=func, **kw), reads=reads, writes=writes)

        def TS(eng, out, in0, s1, s2, op0, op1, reads, writes):
            if s2 is None:
                S.op(eng, lambda e: e.tensor_scalar(out=out, in0=in0, scalar1=s1, scalar2=None, op0=op0),
                     reads=reads, writes=writes)
            else:
                S.op(eng, lambda e: e.tensor_scalar(out=out, in0=in0, scalar1=s1, scalar2=s2, op0=op0, op1=op1),
                     reads=reads, writes=writes)

        def TT(eng, out, in0, in1, op, reads, writes):
            S.op(eng, lambda e: e.tensor_tensor(out=out, in0=in0, in1=in1, op=op), reads=reads, writes=writes)

        def STT(out, in0, scalar, in1, op0, op1, reads, writes):
            S.op("dve", lambda e: e.scalar_tensor_tensor(out=out, in0=in0, scalar=scalar, in1=in1, op0=op0, op1=op1),
                 reads=reads, writes=writes)

        def CP(eng, out, in_, reads, writes):
            if eng == "act":
                S.op(eng, lambda e: e.copy(out=out, in_=in_), reads=reads, writes=writes)
            else:
                S.op(eng, lambda e: e.tensor_copy(out=out, in_=in_), reads=reads, writes=writes)

        def MEMSET(eng, ap, v, writes):
            S.op(eng, lambda e: e.memset(ap, v), writes=writes)

        def DMA(eng, out, in_, reads, writes, key):
            S.op(eng, lambda e: e.dma_start(out=out, in_=in_), reads=reads, writes=writes, dma=key)

        foff = [0]
        boff = [0]

        def fr(n):
            o = foff[0]
            foff[0] += n
            assert foff[0] <= FSZ, foff[0]
            return Freg[:, o:o + n]

        def br(n):
            o = boff[0]
            boff[0] += n
            assert boff[0] <= BSZ, boff[0]
            return Breg[:, o:o + n]

        lnv = fr(6 * 1024).rearrange("p (a d) -> p a d", a=6)
        b_lnv = Buf("lnv")
        HX = [[fr(1024) for _ in range(2)] for _ in range(2)]
        b_HX = [[Buf(f"hx{p}{s}") for s in range(2)] for p in range(2)]
        xin = HX[0]
        b_xin = b_HX[0]
        vg = [fr(1024) for _ in range(2)]
        b_vg = [Buf(f"vg{s}") for s in range(2)]
        lru = []
        mh4 = fr(1024)
        for par in range(4):
            d_ = dict(xl=fr(260), xc=fr(256), ra=fr(256), ib=fr(256), mh=mh4[:, par * 256:(par + 1) * 256])
            d_["B"] = {k: Buf(f"lru{par}{k}") for k in ("xl", "xc", "ra", "ib", "mh")}
            lru.append(d_)
        b_glb = [Buf(f"glb{i}") for i in range(8)]
        sig = [fr(256) for _ in range(2)]
        b_sig = [Buf(f"sig{k}") for k in range(2)]
        prod = [fr(256) for _ in range(2)]
        b_prod = [Buf(f"prod{k}") for k in range(2)]
        macc_off = foff[0]
        macc = [fr(256) for _ in range(4)]
        b_macc = [Buf(f"macc{k}") for k in range(4)]
        rden = fr(256)
        b_rden = Buf("rden")
        r1 = [fr(1024) for _ in range(2)]
        b_r1 = [Buf(f"r1_{i}") for i in range(2)]
        h1t = fr(1024)
        b_h1t = Buf("h1t")
        h1Tf = fr(1024).rearrange("p (k t) -> p k t", k=8)
        b_h1Tf = Buf("h1Tf")

        def stats_set(name):
            return dict(st=fr(24), mv=fr(4), rs=fr(2), B=Buf(name))
        ss_in = [stats_set(f"ss_in{s}") for s in range(2)]
        ss_v = [stats_set(f"ss_v{s}") for s in range(2)]
        ss_1 = stats_set("ss_1")
        ss_2 = stats_set("ss_2")
        rt = dict(s=fr(64), ch=fr(64), o8=fr(64), gs=fr(8), srt=fr(8), gm=fr(8), nbg=fr(8), msk=fr(64), t8=fr(8),
                  sel=fr(64), wun=fr(64), den=fr(2), rd=fr(2), cnt=fr(64), valid=fr(64), dall=fr(64), wv=fr(64),
                  csum=fr(64), csel=fr(64), dstf=fr(8))
        rt["oh"] = vg[0][:, 0:512]
        rt["pr"] = vg[0][:, 512:1024]
        b_rt = {k: Buf("rt_" + k) for k in rt}
        f_mixer_end = foff[0]

        h0Tp = [br(8 * T).rearrange("p (k t) -> p k t", k=8) for _ in range(2)]
        b_h0Tp = [Buf(f"h0T{p}") for p in range(2)]
        ybr0p = [br(8 * T).rearrange("p (k t) -> p k t", k=8) for _ in range(2)]
        b_ybr0p = [[Buf(f"y0{p}_{c}") for c in range(8)] for p in range(2)]
        glb = [br(T) for _ in range(8)]
        vn = br(2 * 1024).rearrange("p (s d) -> p s d", s=2)
        b_vn = [Buf(f"vn{s}") for s in range(2)]
        ub = [br(T) for _ in range(2)]
        b_ub = [Buf(f"u{i}") for i in range(2)]
        ybr = [None] + [br(8 * T).rearrange("p (k t) -> p k t", k=8) for _ in range(2)]
        b_ybr = [None] + [[Buf(f"y{k}_{c}") for c in range(8)] for k in (1, 2)]
        xcb = [br(T) for _ in range(4)]
        b_xcb = [Buf(f"xcb{i}") for i in range(4)]
        qT = br(8 * T).rearrange("p (k t) -> p k t", k=8)
        b_qT = [Buf(f"qT{c}") for c in range(8)]
        expT = [br(2 * T).rearrange("p (m t) -> p m t", m=2) for _ in range(2)]
        b_expT = [Buf(f"expT{i}") for i in range(2)]
        kT = br(8 * 256).rearrange("p (k t) -> p k t", k=8)
        b_kT = Buf("kT")
        vv = br(2 * 1024).rearrange("p (m d) -> p m d", m=2)
        b_vv = Buf("vv")
        memT = None
        mrg = br(8 * T).rearrange("p (k t) -> p k t", k=8)
        b_mrg = [Buf(f"mrg{c}") for c in range(8)]
        memT = qT
        h1Tb = br(8 * T).rearrange("p (k t) -> p k t", k=8)
        b_h1Tb = Buf("h1Tb")
        h1b = [br(1024) for _ in range(2)]
        b_h1b = [Buf(f"h1b{i}") for i in range(2)]
        selb = br(64)
        b_selb = Buf("selb")
        b_Xs = Buf("Xs")
        b_Ys = Buf("Ys")
        b_ews = Buf("ews")
        b_dst = Buf("dst_all")
        b_wk = Buf("wk_all")
        b_carry = Buf("carry")

        wslv = [wsl[:, i, :].rearrange("p (k c) -> p k c", k=8) for i in range(4)]
        b_wsl = [Buf(f"wsl{i}") for i in range(4)]
        wsl_ctr = [0]
        b_wden = Buf("wden")
        b_ln2v = Buf("ln2v")
        b_const = Buf("const")
        b_halo = Buf("halo")
        b_hst = Buf("hstate")
        b_wsc = Buf("wsc")

        blk_id = {}
        nblk = [0]

        b_wscB = Buf("wscB")
        blk_buf = {}

        pre_bufs = {}

        def precast(name, src, cbs, grp):
            for cb in cbs:
                n = nblk[0]
                blk_id[(name, cb)] = n
                grp_i = n // 4
                if grp_i not in pre_bufs:
                    pre_bufs[grp_i] = Buf(f"wscg{grp_i}")
                bb = pre_bufs[grp_i]
                blk_buf[(name, cb)] = bb
                DMA("pool", wsc[n].rearrange("p (k c) -> p k c", k=8),
                    src[:, cb * 512:(cb + 1) * 512].rearrange("(k p) c -> p k c", p=128), [], [], f"d_pre{grp_i}")
                bb.w = (f"d_pre{grp_i}", S.dma_cnt[f"d_pre{grp_i}"])
                nblk[0] += 1
        precast("kv", w_kv_d, range(4), 0)
        precast("in", w_in_d, [2, 3, 0, 1, 6, 7, 4, 8, 5, 9], 0)
        for jb in range(2):
            for k in range(3):
                precast("in", w_in_d, [10 + 2 * k + jb], 1)
                precast(f"br{k}", w_br_d[k], [jb], 1)
        precast("out", w_out_d, range(2), 1)
        def precast_expert(e):
            if e < NEXP:
                g_src, u_src, d_src = eg_d[e], eu_d[e], ed_d[e]
            else:
                g_src, u_src, d_src = sg_d, su_d, sd_d
            gu = ews[e][:, 0:4096].rearrange("p (k c) -> p k c", k=8)
            DMA("pool", gu[:, :, 0:256], g_src.rearrange("(k p) f -> p k f", p=128), [], [], "d_pre2")
            DMA("pool", gu[:, :, 256:512], u_src.rearrange("(k p) f -> p k f", p=128), [], [], "d_pre2")
            DMA("pool", ews[e][:, 4096:6144].rearrange("p (f d) -> p f d", f=2),
                d_src.rearrange("(f p) d -> p f d", p=128), [], [], "d_pre2")
            b_ews.w = ("d_pre2", S.dma_cnt["d_pre2"])

        def wload(name, cb, slot=None):
            if slot is None:
                i = wsl_ctr[0] % 4
                wsl_ctr[0] += 1
            else:
                i = slot
            DMA("sp", wsl[:, i, :], wsc[blk_id[(name, cb)]], [blk_buf[(name, cb)]], [b_wsl[i]], f"d_wsl{i}")
            return i

        DMA("sp", ident[:], ident_d, [], [b_ident], "d_c0")
        DMA("sp", cvec[:], cvec_d, [], [b_const], "d_c1")
        DMA("sp", wr[:], wr_d, [], [b_const], "d_c1")
        DMA("sp", rbias[:], rbias_d, [], [b_const], "d_c1")
        DMA("sp", lnv, lnvec_d[:, 0:6, :], [], [b_lnv], "d_c3")
        MEMSET("pool", bspf[:], 0.0, [b_const])
        DMA("pool", bspf[0:1, :], bsp_d, [], [b_const], "d_c4")
        DMA("pool", wa[:], wa_d, [], [b_const], "d_c4")
        DMA("pool", wi[:], wi_d, [], [b_const], "d_c4")
        MEMSET("dve", ones_bf[:], 1.0, [b_const])
        MEMSET("dve", ones64[:], 1.0, [b_const])
        MEMSET("dve", carry[:], 0.0, [b_carry])
        DMA("sp", iotaC[:], iotaC_d, [], [b_const], "d_c1")
        DMA("sp", dummy[:], dummy_d, [], [b_const], "d_c1")
        DMA("sp", kvals[:], kvals_d, [], [b_const], "d_c1")
        DMA("pool", ustr_bf[:], ustrict_d, [], [b_const], "d_c4")
        DMA("pool", ident_bf[:], ident_d, [], [b_const], "d_c4")
        MEMSET("pool", h1b[0], 0.0, [b_h1b[0]])
        DMA("sp", Ys[NSLOT:NSLOT + 128, :], h1b[0], [b_h1b[0]], [b_Ys], "d_ysz")
        stg = xin[0].rearrange("p (g t) -> p g t", g=8)
        DMA("sp", stg, wspT_d, [], [b_xin[0]], "d_xin0")
        DMA("sp", xin[1][:, 0:128], maskT_d, [], [b_xin[1]], "d_xin1")
        for g in range(8):
            TT("dve", wspT[:, g, :], stg[:, g, :], xin[1][:, 0:128], ALU.mult, [b_xin[0], b_xin[1]], [b_const])
        ACT(cch[:], cvec[:, 56:64], AF.Exp, [b_const], [b_const], scale=-1.0)
        ACT(cch[:], cch[:], AF.Ln, [b_const], [b_const], bias=1.0)
        TS("dve", cch[:], cch[:], -8.0, None, ALU.mult, None, [b_const], [b_const])
        TS("dve", chf[:, 0:8], cch[:], 0.5, None, ALU.mult, None, [b_const], [b_const])
        TS("dve", chf[:, 8:24], cvec[:, 40:56], 0.5, None, ALU.mult, None, [b_const], [b_const])
        MEMSET("dve", cpow[:, 0:1], -0.5, [b_const])
        MEMSET("dve", cpow[:, 1:2], 0.5, [b_const])

        cw = lambda k, c: cvec[:, k * 8 + c:k * 8 + c + 1]
        cb_ = lambda c: cvec[:, 32 + c:33 + c]
        cba = lambda c: cvec[:, 40 + c:41 + c]
        cbi = lambda c: cvec[:, 48 + c:49 + c]

        def layer_norm_multi(items, g_ap, b_ap, b_gb, ss, eps=LN_EPS):
            n = len(items)
            B = ss["B"]
            st = ss["st"].rearrange("p (j a b) -> p j a b", j=2, a=2)
            mv = ss["mv"].rearrange("p (j c) -> p j c", j=2)
            for j, (src, b_src, dst, b_dst) in enumerate(items):
                for h in range(2):
                    S.op("dve", lambda e, j=j, h=h, src=src: e.bn_stats(out=st[:, j, h, :], in_=src[:, h * 512:(h + 1) * 512]),
                         reads=[b_src], writes=[B])
                S.op("dve", lambda e, j=j: e.bn_aggr(out=mv[:, j, :], in_=st[:, j, :, :].rearrange("p a b -> p (a b)")),
                     reads=[B], writes=[B])
            ACT(ss["rs"][:, 0:n], mv[:, 0:n, 1], AF.Sqrt, [B], [B], bias=float(eps), scale=1.0)
            S.op("dve", lambda e: e.reciprocal(out=ss["rs"][:, 0:n], in_=ss["rs"][:, 0:n]), reads=[B], writes=[B])
            for j, (src, b_src, dst, b_dst) in enumerate(items):
                STT(src, src, mv[:, j, 0:1], g_ap, ALU.subtract, ALU.mult, [b_src, B, b_gb], [b_src])
                STT(dst, src, ss["rs"][:, j:j + 1], b_ap, ALU.mult, ALU.add, [b_src, B, b_gb], [b_dst])

        def layer_norm(src, b_src, dst, b_dst, g_ap, b_ap, b_gb, ss, eps=LN_EPS):
            layer_norm_multi([(src, b_src, dst, b_dst)], g_ap, b_ap, b_gb, ss, eps)

        def kv_precompute(seq):
            for mt in range(2):
                DMA("sp", vg[mt], mem_d[seq * 256 + mt * 128: seq * 256 + (mt + 1) * 128, :], [], [b_vg[mt]], f"d_vg{mt}")
            for mt in range(2):
                for hf in range(2):
                    bk = nb()
                    for c in range(4):
                        cc = hf * 4 + c
                        TR(pb[bk][:, c * 128:(c + 1) * 128], vg[mt][:, cc * 128:(cc + 1) * 128], [b_vg[mt]], bk)
                    CP("act" if hf else "dve", memT[:, hf * 4:(hf + 1) * 4, mt * 128:(mt + 1) * 128],
                       pb[bk][:].rearrange("p (a b) -> p a b", a=4), [Pb[bk]], b_qT)
            for jb in range(2):
                si = wload("kv", jb)
                for j4 in range(4):
                    jc = jb * 4 + j4
                    bk = nb()
                    for k in range(8):
                        MM(pb[bk][:, 0:256], wslv[si][:, k, j4 * 128:(j4 + 1) * 128], memT[:, k, :], k == 0, k == 7,
                           [b_wsl[si]] + b_qT, bk)
                    CP("act" if j4 % 2 else "dve", kT[:, jc, :], pb[bk][:, 0:256], [Pb[bk]], [b_kT])
            for hb in range(2):
                si = wload("kv", 2 + hb)
                for mt in range(2):
                    bk = nb()
                    for k in range(8):
                        MM(pb[bk][:], memT[:, k, mt * 128:(mt + 1) * 128], wslv[si][:, k, :], k == 0, k == 7,
                           [b_wsl[si]] + b_qT, bk)
                    CP("act" if mt else "dve", vv[:, mt, hb * 512:(hb + 1) * 512], pb[bk][:], [Pb[bk]], [b_vv])

        def zfeat(si, j4, bk, par):
            for k in range(8):
                MM(pb[bk][:, 0:T], wslv[si][:, k, j4 * 128:(j4 + 1) * 128], h0Tp[par][:, k, :], k == 0, k == 7,
                   [b_wsl[si], b_h0Tp[par]], bk)

        def route(tile_idx):
            R = rt
            bk = nb("S")
            for k in range(8):
                MM(pb[bk][:, 0:64], h1Tf[:, k, :], wr[:, k, :], k == 0, k == 7, [b_h1Tf, b_const], bk)
            ACT(R["s"], pb[bk][:, 0:64], AF.Tanh, [Pb[bk]], [b_rt["s"]], scale=0.5)
            TS("dve", R["s"], R["s"], 0.5, 0.5, ALU.mult, ALU.add, [b_rt["s"]], [b_rt["s"]])
            TT("dve", R["ch"], R["s"], rbias[:], ALU.add, [b_rt["s"], b_const], [b_rt["ch"]])
            for g in range(8):
                S.op("dve", lambda e, g=g: e.max(out=R["o8"][:, g * 8:(g + 1) * 8], in_=R["ch"][:, g * 8:(g + 1) * 8]),
                     reads=[b_rt["ch"]], writes=[b_rt["o8"]])
            o8v = R["o8"].rearrange("p (g j) -> p g j", g=8)
            TT("dve", R["gs"], o8v[:, :, 0], o8v[:, :, 1], ALU.add, [b_rt["o8"]], [b_rt["gs"]])
            S.op("dve", lambda e: e.max(out=R["srt"], in_=R["gs"]), reads=[b_rt["gs"]], writes=[b_rt["srt"]])
            yield
            TS("dve", R["gm"], R["gs"], R["srt"][:, 3:4], None, ALU.is_ge, None, [b_rt["gs"], b_rt["srt"]], [b_rt["gm"]])
            TS("dve", R["nbg"], R["gm"], -1.0, 1e9, ALU.add, ALU.mult, [b_rt["gm"]], [b_rt["nbg"]])
            for g in range(8):
                TS("dve", R["msk"][:, g * 8:(g + 1) * 8], R["ch"][:, g * 8:(g + 1) * 8], R["gm"][:, g:g + 1],
                   R["nbg"][:, g:g + 1], ALU.mult, ALU.add, [b_rt["ch"], b_rt["gm"], b_rt["nbg"]], [b_rt["msk"]])
            yield
            S.op("dve", lambda e: e.max(out=R["t8"], in_=R["msk"]), reads=[b_rt["msk"]], writes=[b_rt["t8"]])
            TS("dve", R["sel"], R["msk"], R["t8"][:, 7:8], None, ALU.is_ge, None, [b_rt["msk"], b_rt["t8"]], [b_rt["sel"]])
            TT("dve", R["wun"], R["s"], R["sel"], ALU.mult, [b_rt["s"], b_rt["sel"]], [b_rt["wun"]])
            S.op("dve", lambda e: e.reduce_sum(out=R["den"][:, 0:1], in_=R["wun"], axis=AX.X),
                 reads=[b_rt["wun"]], writes=[b_rt["den"]])
            S.op("dve", lambda e: e.reciprocal(out=R["rd"][:, 0:1], in_=R["den"][:, 0:1]),
                 reads=[b_rt["den"]], writes=[b_rt["rd"]])
            TS("dve", wden[:, 0, :], R["wun"], R["rd"][:, 0:1], 2.5, ALU.mult, ALU.mult,
               [b_rt["wun"], b_rt["rd"]], [b_wden])
            for _ in range(3):
                yield
            CP("pool", selb, R["sel"], [b_rt["sel"]], [b_selb])
            bk = nb("S")
            MM(pb[bk][:, 0:64], ustr_bf[:], selb, True, True, [b_const, b_selb], bk)
            MM(pb[bk][:, 64:128], ones_bf[:], selb, True, True, [b_const, b_selb], bk)
            TT("dve", R["cnt"], pb[bk][:, 0:64], carry[:], ALU.add, [Pb[bk], b_carry], [b_rt["cnt"]])
            TT("dve", carry[:], pb[bk][:, 64:128], carry[:], ALU.add, [Pb[bk], b_carry], [b_carry])
            TS("dve", R["valid"], R["cnt"], float(CAP), None, ALU.is_lt, None, [b_rt["cnt"]], [b_rt["valid"]])
            TT("dve", R["dall"], R["cnt"], iotaC[:], ALU.add, [b_rt["cnt"], b_const], [b_rt["dall"]])
            STT(R["dall"], R["dall"], dummy[:, 0:1], R["valid"], ALU.subtract, ALU.mult,
                [b_rt["dall"], b_rt["valid"], b_const], [b_rt["dall"]])
            TS("dve", R["dall"], R["dall"], dummy[:, 0:1], None, ALU.add, None, [b_rt["dall"], b_const], [b_rt["dall"]])
            yield
            TT("dve", R["wv"], wden[:, 0, :], R["valid"], ALU.mult, [b_wden, b_rt["valid"]], [b_rt["wv"]])
            S.op("dve", lambda e: e.tensor_tensor_scan(out=R["csum"], data0=ones64[:], data1=R["sel"], initial=0.0,
                                                       op0=ALU.mult, op1=ALU.add),
                 reads=[b_const, b_rt["sel"]], writes=[b_rt["csum"]])
            TT("dve", R["csel"], R["csum"], R["sel"], ALU.mult, [b_rt["csum"], b_rt["sel"]], [b_rt["csel"]])
            oh3 = R["oh"].rearrange("p (k e) -> p k e", k=8)
            pr3 = R["pr"].rearrange("p (k e) -> p k e", k=8)
            bc = lambda ap: ap.unsqueeze(1).broadcast_to([128, 8, 64])
            yield
            B_oh = [b_vg[0]]
            B_pr = [b_vg[0]]
            TT("dve", oh3, kvals[:].unsqueeze(2).broadcast_to([128, 8, 64]), bc(R["csel"]), ALU.is_equal,
               [b_const, b_rt["csel"]], B_oh)
            TT("dve", pr3, oh3, bc(R["dall"]), ALU.mult, B_oh + [b_rt["dall"]], B_pr)
            S.op("dve", lambda e: e.reduce_sum(out=R["dstf"], in_=pr3, axis=AX.X), reads=B_pr, writes=[b_rt["dstf"]])
            CP("dve", dst_all[:, tile_idx, :], R["dstf"], [b_rt["dstf"]], [b_dst])
            TT("dve", pr3, oh3, bc(R["wv"]), ALU.mult, B_oh + [b_rt["wv"]], B_pr)
            S.op("dve", lambda e: e.reduce_sum(out=wk_all[:, tile_idx, :], in_=pr3, axis=AX.X),
                 reads=B_pr, writes=[b_wk])

        b_r1s = [Buf(f"r1s{i}") for i in range(32)]
        b_h1Ts = [Buf(f"h1Ts{i}") for i in range(2)]
        per = -(-(NEXP + 1) // n_tiles)

        def xload(ti):
            for s in range(2):
                t0 = ti * T + s * 128
                DMA("sp", HX[ti % 2][s], x_d[t0:t0 + 128, :], [], [b_HX[ti % 2][s]], f"d_xin{ti % 2}{s}")

        def stage_A(ti):
            par = ti % 2
            h0, b_h0 = HX[par], b_HX[par]
            seq = (ti * T) // SEQ
            if (ti * T) % SEQ == 0:
                kv_precompute(seq)
                MEMSET("pool", halo[:], 0.0, [b_halo])
                MEMSET("pool", hstate[:], 0.0, [b_hst])
            layer_norm_multi([(h0[s], b_h0[s], h0[s], b_h0[s]) for s in range(2)], lnv[:, 0, :], lnv[:, 1, :], b_lnv, ss_in[0])
            yield
            for s in range(2):
                for hf in range(2):
                    bk = nb("S")
                    for c in range(4):
                        cc = hf * 4 + c
                        TR(pb[bk][:, c * 128:(c + 1) * 128], h0[s][:, cc * 128:(cc + 1) * 128], [b_h0[s]], bk)
                    CP("act" if hf else "dve", h0Tp[par][:, hf * 4:(hf + 1) * 4, s * 128:(s + 1) * 128],
                       pb[bk][:].rearrange("p (a b) -> p a b", a=4), [Pb[bk]], [b_h0Tp[par]])
                yield

        def stage_B(ti):
            par = ti % 2
            hT, bhT = h0Tp[par], b_h0Tp[par]
            for hb in range(2):
                si = wload("in", 2 + hb, 2 * hb)
                for s in range(2):
                    bk = nb("S")
                    for k in range(8):
                        MM(pb[bk][:], hT[:, k, s * 128:(s + 1) * 128], wslv[si][:, k, :], k == 0, k == 7,
                           [b_wsl[si], bhT], bk)
                    ACT(vg[s][:, hb * 512:(hb + 1) * 512], pb[bk][:], AF.Gelu_apprx_tanh, [Pb[bk]], [b_vg[s]])
                yield
            layer_norm_multi([(vg[s], b_vg[s], vn[:, s, :], b_vn[s]) for s in range(2)], lnv[:, 2, :], lnv[:, 3, :], b_lnv, ss_v[0])
            yield
            for jb in range(2):
                si = wload("in", jb, 2 * jb)
                for j4 in range(4):
                    g = jb * 4 + j4
                    bk = nb("S")
                    zfeat(si, j4, bk, par)
                    ACT(ub[g % 2], pb[bk][:, 0:T], AF.Gelu_apprx_tanh, [Pb[bk]], [b_ub[g % 2]])
                    bk2 = nb("S")
                    for s in range(2):
                        MM(pb[bk2][:, s * 128:(s + 1) * 128], ones_bf[:], bspf[:, g * 128:(g + 1) * 128],
                           True, False, [b_const], bk2)
                        MM(pb[bk2][:, s * 128:(s + 1) * 128], vn[:, s, g * 128:(g + 1) * 128], wspT[:, g, :],
                           False, True, [b_const, b_vn[s]], bk2)
                    TT("dve", ybr0p[par][:, g, :], ub[g % 2], pb[bk2][:, 0:T], ALU.mult, [b_ub[g % 2], Pb[bk2]], [b_ybr0p[par][g]])
                yield

        def stage_B2(ti):
            par = ti % 2
            for jb in range(2):
                si = wload("in", 6 + jb)
                for j4 in range(4):
                    g = jb * 4 + j4
                    bk = nb("S")
                    zfeat(si, j4, bk, par)
                    ACT(glb[g], pb[bk][:, 0:T], AF.Gelu_apprx_tanh, [Pb[bk]], [b_glb[g]])
                yield

        def stage_C(ti):
            hcc = lambda g: chf[:, g:g + 1]
            hba = lambda g: chf[:, 8 + g:9 + g]
            hbi = lambda g: chf[:, 16 + g:17 + g]
            for jb in range(2):
                si_x = wload("in", 4 + jb, 1)
                G4 = [jb * 4 + c for c in range(4)]
                bx = {}
                for c4, g in enumerate(G4):
                    bx[c4] = nb("C")
                    zfeat(si_x, c4, bx[c4], ti % 2)
                yield
                for c4, g in enumerate(G4):
                    L = lru[c4]
                    LB = L["B"]
                    CP("pool", L["xl"][:, 0:3], halo[:, g, 0:3], [b_halo], [LB["xl"]])
                    CP("act", L["xl"][:, 3:3 + T], pb[bx[c4]][:, 0:T], [Pb[bx[c4]]], [LB["xl"]])
                    CP("pool", halo[:, g, 0:3], L["xl"][:, T:T + 3], [LB["xl"]], [b_halo])
                yield
                for c4, g in enumerate(G4):
                    L = lru[c4]
                    LB = L["B"]
                    ACT(L["xc"], L["xl"][:, 3:3 + T], AF.Identity, [LB["xl"], b_const], [LB["xc"]], bias=cb_(g), scale=cw(0, g))
                yield
                for k in range(1, 4):
                    for c4, g in enumerate(G4):
                        L = lru[c4]
                        LB = L["B"]
                        STT(L["xc"], L["xl"][:, 3 - k:3 - k + T], cw(k, g), L["xc"], ALU.mult, ALU.add,
                            [LB["xl"], LB["xc"], b_const], [LB["xc"]])
                    yield
                for c4, g in enumerate(G4):
                    L = lru[c4]
                    CP("act", xcb[c4], L["xc"], [L["B"]["xc"]], [b_xcb[c4]])
                yield
                b1 = {}
                for c4, g in enumerate(G4):
                    b1[c4] = nb("C")
                    MM(pb[b1[c4]][:, 0:T], wa[:, g, :], xcb[c4], True, True, [b_const, b_xcb[c4]], b1[c4])
                    MM(pb[b1[c4]][:, T:2 * T], wi[:, g, :], xcb[c4], True, True, [b_const, b_xcb[c4]], b1[c4])
                yield
                for c4, g in enumerate(G4):
                    L = lru[c4]
                    ACT(L["ra"], pb[b1[c4]][:, 0:T], AF.Tanh, [Pb[b1[c4]], b_const], [L["B"]["ra"]], bias=hba(g), scale=0.5)
                    ACT(L["ib"], pb[b1[c4]][:, T:2 * T], AF.Tanh, [Pb[b1[c4]], b_const], [L["B"]["ib"]], bias=hbi(g), scale=0.5)
                yield
                for c4, g in enumerate(G4):
                    L = lru[c4]
                    ACT(L["ra"], L["ra"], AF.Exp, [L["B"]["ra"], b_const], [L["B"]["ra"]], bias=hcc(g), scale=hcc(g))
                yield
                for c4, g in enumerate(G4):
                    L = lru[c4]
                    LB = L["B"]
                    STT(L["ib"], L["ib"], 1.0, L["xc"], ALU.add, ALU.mult, [LB["ib"], LB["xc"]], [LB["ib"]])
                    TT("pool", L["mh"], L["ra"], L["ra"], ALU.mult, [LB["ra"]], [LB["mh"]])
                yield
                mhB = [lru[c4]["B"]["mh"] for c4 in range(4)]
                ACT(mh4, mh4, AF.Sqrt, mhB, mhB, bias=0.25, scale=-0.25)
                yield
                for c4, g in enumerate(G4):
                    L = lru[c4]
                    LB = L["B"]
                    TT("pool", L["ib"], L["ib"], L["mh"], ALU.mult, [LB["ib"], LB["mh"]], [LB["ib"]])
                yield
                for c4, g in enumerate(G4):
                    L = lru[c4]
                    LB = L["B"]
                    S.op("dve", lambda e, L=L, g=g: e.tensor_tensor_scan(out=L["mh"], data0=L["ra"], data1=L["ib"],
                                                                         initial=hstate[:, g:g + 1], op0=ALU.mult, op1=ALU.add),
                         reads=[LB["ra"], LB["ib"], b_hst], writes=[LB["mh"]])
                yield
                for c4, g in enumerate(G4):
                    L = lru[c4]
                    LB = L["B"]
                    CP("pool", hstate[:, g:g + 1], L["mh"][:, T - 1:T], [LB["mh"]], [b_hst])
                    TT("pool", ybr[1][:, g, :], L["mh"], glb[g], ALU.mult, [LB["mh"], b_glb[g]], [b_ybr[1][g]])
                yield

        def stage_D(ti):
            for jb in range(2):
                si = wload("in", 8 + jb, 3)
                for j4 in range(4):
                    j = jb * 4 + j4
                    bk = nb("D")
                    zfeat(si, j4, bk, ti % 2)
                    CP("act", qT[:, j, :], pb[bk][:, 0:T], [Pb[bk]], [b_qT[j]])
                    yield
            for hh in range(4):
                bS = nb("D")
                for mc in range(2):
                    for dc in range(2):
                        MM(pb[bS][:, mc * T:(mc + 1) * T], kT[:, 2 * hh + dc, mc * 128:(mc + 1) * 128], qT[:, 2 * hh + dc, :],
                           dc == 0, dc == 1, [b_kT, b_qT[2 * hh + dc]], bS)
                ex = expT[hh % 2]
                yield
                ACT(ex, pb[bS][:].rearrange("p (m t) -> p m t", m=2), AF.Exp, [Pb[bS]], [b_expT[hh % 2]], scale=1.0 / 16.0)
                bD = nb("D")
                for mc in range(2):
                    MM(pb[bD][:, 0:T], ones_bf[:], ex[:, mc, :], mc == 0, mc == 1, [b_const, b_expT[hh % 2]], bD)
                bO = nb("D")
                for dc in range(2):
                    for mc in range(2):
                        MM(pb[bO][:, dc * T:(dc + 1) * T], vv[:, mc, hh * 256 + dc * 128: hh * 256 + (dc + 1) * 128],
                           ex[:, mc, :], mc == 0, mc == 1, [b_vv, b_expT[hh % 2]], bO)
                yield
                S.op("dve", lambda e, bD=bD: e.reciprocal(out=rden, in_=pb[bD][:, 0:T]), reads=[Pb[bD]], writes=[b_rden])
                for dc in range(2):
                    TT("dve", ybr[2][:, 2 * hh + dc, :], pb[bO][:, dc * T:(dc + 1) * T], rden, ALU.mult,
                       [Pb[bO], b_rden], [b_ybr[2][2 * hh + dc]])

        def stage_E(ti):
            pc = 0
            for jb in range(2):
                for k in range(3):
                    sgt = wload("in", 10 + 2 * k + jb)
                    sbr = wload(f"br{k}", jb)
                    for j4 in range(4):
                        j = jb * 4 + j4
                        bk = nb()
                        zfeat(sgt, j4, bk, ti % 2)
                        sg_i = pc % 2
                        pc += 1
                        ACT(sig[sg_i], pb[bk][:, 0:T], AF.Tanh, [Pb[bk]], [b_sig[sg_i]], scale=0.5)
                        bkp = nb()
                        for c in range(8):
                            yk = ybr0p[ti % 2] if k == 0 else ybr[k]
                            byk = b_ybr0p[ti % 2] if k == 0 else b_ybr[k]
                            MM(pb[bkp][:, 0:T], wslv[sbr][:, c, j4 * 128:(j4 + 1) * 128], yk[:, c, :], c == 0, c == 7,
                               [b_wsl[sbr], byk[c]], bkp)
                        if k == 0:
                            STT(macc[j4], sig[sg_i], 1.0, pb[bkp][:, 0:T], ALU.add, ALU.mult, [b_sig[sg_i], Pb[bkp]], [b_macc[j4]])
                        else:
                            STT(prod[sg_i], sig[sg_i], 1.0, pb[bkp][:, 0:T], ALU.add, ALU.mult, [b_sig[sg_i], Pb[bkp]], [b_prod[sg_i]])
                            if k == 1:
                                TT("pool", macc[j4], macc[j4], prod[sg_i], ALU.add, [b_macc[j4], b_prod[sg_i]], [b_macc[j4]])
                            else:
                                TT("pool", mrg[:, j, :], macc[j4], prod[sg_i], ALU.add, [b_macc[j4], b_prod[sg_i]], [b_mrg[j]])
                    yield

        def stage_F(ti):
            so = [wload("out", 0), wload("out", 1)]
            h0, b_h0 = HX[ti % 2], b_HX[ti % 2]
            for s in range(2):
                for hb in range(2):
                    bk = nb()
                    for k in range(8):
                        MM(pb[bk][:], mrg[:, k, s * 128:(s + 1) * 128], wslv[so[hb]][:, k, :], k == 0, k == 7,
                           [b_wsl[so[hb]], b_mrg[k]], bk)
                    STT(r1[s][:, hb * 512:(hb + 1) * 512], h0[s][:, hb * 512:(hb + 1) * 512], 2.0 * ALPHA, pb[bk][:], ALU.mult, ALU.add,
                        [b_h0[s], Pb[bk]], [b_r1[s]])
            if debug and ti == 0:
                for k in range(3):
                    yk = ybr0p[ti % 2] if k == 0 else ybr[k]
                    byk = b_ybr0p[ti % 2] if k == 0 else b_ybr[k]
                    DMA("sp", dbg_y[k].rearrange("p (k t) -> p k t", k=8), yk, [byk[c] for c in range(8)], [], "d_dbg")
                DMA("sp", dbg_y[3].rearrange("p (k t) -> p k t", k=8), mrg, b_mrg, [], "d_dbg")

        def stage_G(ti, s):
            t0 = ti * T + s * 128
            layer_norm(r1[s], b_r1[s], h1t, b_h1t, lnv[:, 4, :], lnv[:, 5, :], b_lnv, ss_1, eps=4.0 * LN_EPS)
            for _ in range(3):
                yield
            ACT(r1[s], h1t, AF.Copy, [b_h1t], [b_r1[s]], scale=ALPHA)
            DMA("act", r1s[t0:t0 + 128, :], r1[s], [b_r1[s]], [b_r1s[ti * 2 + s]], f"d_r1{s}")
            for hf in range(2):
                bk = nb("S")
                for c in range(4):
                    cc = hf * 4 + c
                    TR(pb[bk][:, c * 128:(c + 1) * 128], h1t[:, cc * 128:(cc + 1) * 128], [b_h1t], bk)
                CP("act", h1Tf[:, hf * 4:(hf + 1) * 4, :], pb[bk][:].rearrange("p (a b) -> p a b", a=4), [Pb[bk]], [b_h1Tf])
            yield
            CP("act", h1Tb[:, :, s * 128:(s + 1) * 128], h1Tf, [b_h1Tf], [b_h1Tb])
            yield from route(ti * 2 + s)
            hp = (ti * 2 + s) % 2
            CP("act", h1b[hp], h1t, [b_h1t], [b_h1b[hp]])
            for k in range(8):
                tix = ti * 2 + s
                S.op("pool", lambda e, hp=hp, tix=tix, k=k: e.indirect_dma_start(
                    out=Xs, out_offset=bass.IndirectOffsetOnAxis(ap=dst_all[:, tix, k:k + 1], axis=0),
                    in_=h1b[hp], in_offset=None),
                    reads=[b_h1b[hp], b_dst], writes=[], dma=f"d_sc{hp}")
            if s == 1:
                seq_ = (ti * T) // SEQ
                tpos = (ti * T) % SEQ
                DMA("act", h1Ts[seq_].rearrange("p (k t) -> p k t", k=8)[:, :, tpos:tpos + T], h1Tb, [b_h1Tb], [b_h1Ts[seq_]], "d_h1T")

        def run_interleaved(gens, until=None, weights=None):
            gens = list(gens)
            wts = {id(g_): (weights[i] if weights else 1) for i, g_ in enumerate(gens)}
            while gens:
                if until is not None and not any(g_ in gens for g_ in until):
                    return gens
                for g_ in list(gens):
                    for _ in range(wts[id(g_)]):
                        try:
                            next(g_)
                        except StopIteration:
                            gens.remove(g_)
                            break
            return []

        def G_both(ti):
            yield from stage_G(ti, 0)
            yield from stage_G(ti, 1)

        def AB(ti):
            yield from stage_A(ti)
            yield from stage_B(ti)

        def B2G(ti_next, ti_prev):
            if ti_next < n_tiles:
                yield from stage_B2(ti_next)
            if ti_prev >= 0:
                yield from G_both(ti_prev)

        xload(0)
        if n_tiles > 1:
            xload(1)
        run_interleaved([AB(0)])
        run_interleaved([stage_B2(0)])
        for ti in range(n_tiles):
            nxt = ti + 1
            seq_start_next = nxt < n_tiles and (nxt * T) % SEQ == 0
            gens = [stage_C(ti), stage_D(ti)]
            if nxt < n_tiles and not seq_start_next:
                gens.append(AB(nxt))
            run_interleaved(gens)
            if seq_start_next:
                run_interleaved([AB(nxt)])
            run_interleaved([stage_E(ti), B2G(ti + 1, ti - 1)], weights=[1, 3])
            stage_F(ti)
            if ti + 2 < n_tiles:
                xload(ti + 2)
        run_interleaved([G_both(n_tiles - 1)])

        if debug:
            DMA("sp", dbg_wden, wk_all[:], [b_wk], [], "d_dbg")
            DMA("sp", dbg_dst, dst_all[:], [b_dst], [], "d_dbg")
        S.barrier()
        foff[0] = 0
        boff[0] = 0
        NST = CAP // 128
        xgT = [br(8 * CAP).rearrange("p (k t) -> p k t", k=8) for _ in range(1)]
        b_xgT = [Buf(f"xgT{i}") for i in range(1)]
        NXS = 8
        xs = [br(1024) for _ in range(NXS)]
        b_xs = [Buf(f"xs{i}") for i in range(NXS)]
        sgx = br(2 * 512).rearrange("p (f t) -> p f t", f=2)
        b_sgx = Buf("sgx")
        acx = [br(2 * CAP).rearrange("p (f t) -> p f t", f=2) for _ in range(1)]
        b_acx = [Buf(f"acx{i}") for i in range(1)]
        NYT = 8
        yt = [br(1024) for _ in range(NYT)]
        b_yt = [Buf(f"yt{i}") for i in range(NYT)]
        xs_ctr = [0]
        yt_ctr = [0]
        n_e = NEXP if n_exp > 2 else 2

        wst = [fr(6144) for _ in range(2)]
        b_wst = [Buf(f"wst{i}") for i in range(2)]
        wst_ctr = [0]

        def WL(e, a=None, stage=None):
            if a is None:
                a = 2 * (e % 2)
            if e < NEXP:
                g_src, u_src, d_src = eg_d[e], eu_d[e], ed_d[e]
            else:
                g_src, u_src, d_src = sg_d, su_d, sd_d
            if stage is None:
                wi_ = wst_ctr[0] % 2
                wst_ctr[0] += 1
                W, bW, key = wst[wi_], b_wst[wi_], f"d_wst{wi_}"
            else:
                W, bW, key = stage
            gv = W[:, 0:2048].rearrange("p (k f) -> p k f", k=8)
            uv = W[:, 2048:4096].rearrange("p (k f) -> p k f", k=8)
            dv = W[:, 4096:6144]
            DMA("sp", gv, g_src.rearrange("(k p) f -> p k f", p=128), [], [bW], key)
            DMA("sp", uv, u_src.rearrange("(k p) f -> p k f", p=128), [], [], key)
            DMA("sp", dv.rearrange("p (f d) -> p f d", f=2), d_src.rearrange("(f p) d -> p f d", p=128), [], [],
                key)
            bW.w = (key, S.dma_cnt[key])
            CP("dve", wslv[a][:, :, 0:256], gv, [bW], [b_wsl[a]])
            CP("act", wslv[a][:, :, 256:512], uv, [bW], [b_wsl[a]])
            CP("pool", wsl[:, a + 1, 0:2048], dv, [bW], [b_wsl[a + 1]])

        def TX(e):
            X = xgT[0]
            for st in range(NST):
                xi = xs_ctr[0] % NXS
                xs_ctr[0] += 1
                r0 = e * CAP + st * 128
                DMA("sp", xs[xi], Xs[r0:r0 + 128, :], [], [b_xs[xi]], f"d_xs{xi}")
                bk = nb()
                pbv = pb[bk][:].bitcast(BF16)
                for k in range(8):
                    S.op("pe", lambda e_, xi=xi, k=k, pbv=pbv: e_.transpose(out=pbv[:, k * 128:(k + 1) * 128],
                                                                             in_=xs[xi][:, k * 128:(k + 1) * 128],
                                                                             identity=ident_bf[:]),
                         reads=[b_xs[xi], b_const], writes=[Pb[bk]])
                CP("act" if st % 2 else "dve", X[:, :, st * 128:(st + 1) * 128],
                   pbv.rearrange("p (k t) -> p k t", k=8), [Pb[bk]], [b_xgT[0]])

        def GUX(e):
            a = 2 * (e % 2)
            X = xgT[0]
            groups = [(c0, min(512, CAP - c0)) for c0 in range(0, CAP, 512)]
            for (c0, cn) in groups:
                banks = [nb() for _ in range(4)]
                for fc in range(2):
                    for gu in range(2):
                        bk = banks[2 * gu + fc]
                        for k in range(8):
                            MM(pb[bk][:, 0:cn], wslv[a][:, k, gu * 256 + fc * 128: gu * 256 + (fc + 1) * 128],
                               X[:, k, c0:c0 + cn], k == 0, k == 7, [b_wsl[a], b_xgT[0]], bk)
                for fc in range(2):
                    ACT(sgx[:, fc, 0:cn], pb[banks[fc]][:, 0:cn], AF.Silu, [Pb[banks[fc]]], [b_sgx])
                    TT("dve", acx[0][:, fc, c0:c0 + cn], sgx[:, fc, 0:cn], pb[banks[2 + fc]][:, 0:cn], ALU.mult,
                       [b_sgx, Pb[banks[2 + fc]]], [b_acx[0]])

        def DNX(e):
            a = 2 * (e % 2)
            dn = wsl[:, a + 1, 0:2048].rearrange("p (f d) -> p f d", f=2)
            for st in range(NST):
                yi = yt_ctr[0] % NYT
                yt_ctr[0] += 1
                for hb in range(2):
                    bk = nb()
                    for fc in range(2):
                        MM(pb[bk][:], acx[0][:, fc, st * 128:(st + 1) * 128], dn[:, fc, hb * 512:(hb + 1) * 512],
                           fc == 0, fc == 1, [b_acx[0], b_wsl[a + 1]], bk)
                    CP("act" if hb else "dve", yt[yi][:, hb * 512:(hb + 1) * 512], pb[bk][:], [Pb[bk]], [b_yt[yi]])
                r0 = e * CAP + st * 128
                DMA("pool", Ys[r0:r0 + 128, :], yt[yi], [b_yt[yi]], [], f"d_yt{yi}")

        WL(0)
        WL(1)
        TX(0)
        for e in range(n_e):
            GUX(e)
            if e + 1 < n_e:
                TX(e + 1)
            DNX(e)
            if e + 2 < n_e:
                WL(e + 2)

        S.barrier()
        foff[0] = 0
        boff[0] = 0
        acc = [fr(1024) for _ in range(16)]
        b_acc = [Buf(f"acc{i}") for i in range(16)]
        ss_m = dict(st=fr(24), mv=fr(4), rs=fr(2), B=Buf("ss_m"))
        wst_c = fr(6144)
        b_wst_c = Buf("wst_c")
        ln2v = fr(2048).rearrange("p (a d) -> p a d", a=2)
        DMA("sp", ln2v, lnvec_d[:, 6:8, :], [], [b_ln2v], "d_c2")
        h1T = br(8 * SEQ).rearrange("p (k t) -> p k t", k=8)
        b_h1T = Buf("h1T")
        sgb = [br(1024).rearrange("p (f t) -> p f t", f=2) for _ in range(2)]
        b_sgb = [Buf(f"sgb{i}") for i in range(2)]
        actT = [br(1024).rearrange("p (f t) -> p f t", f=2) for _ in range(2)]
        b_actT = [Buf(f"actT{i}") for i in range(2)]
        gbuf = [br(1024) for _ in range(6)]
        b_gbuf = [Buf(f"gbuf{i}") for i in range(6)]
        g_ctr = [0]
        dg = [br(128) for _ in range(4)]
        b_dg = [Buf(f"dg{i}") for i in range(4)]
        dg_ctr = [0]
        wsp = [dict(bf=br(8), hi=fr(8), lo=fr(8), B=Buf(f"wsp{i}")) for i in range(2)]

        n_seq = max(1, (n_tiles * T) // SEQ)
        for seq in range(n_seq):
            DMA("sp", h1T.rearrange("p k t -> p (k t)"), h1Ts[seq], [b_h1Ts[seq]], [b_h1T], "d_h1Tl")
            for i in range(16):
                t0 = seq * SEQ + i * 128
                DMA("sp", acc[i], r1s[t0:t0 + 128, :], [b_r1s[seq * 16 + i]], [b_acc[i]], f"d_acc{i}")
            steps = [(NEXP, tt) for tt in range(4)]
            a = 0
            if seq == 0:
                WL(NEXP, 0, (wst_c, b_wst_c, "d_wstc"))

            def GU(idx):
                e, tt = steps[idx]
                grp = idx % 2
                par = idx % 2
                for fc in range(2):
                    for gu in range(2):
                        bk = 4 * grp + 2 * gu + fc
                        for k in range(8):
                            MM(pb[bk][:], wslv[a][:, k, gu * 256 + fc * 128: gu * 256 + (fc + 1) * 128],
                               h1T[:, k, tt * 512:(tt + 1) * 512], k == 0, k == 7, [b_wsl[a], b_h1T], bk)
                for fc in range(2):
                    ACT(sgb[par][:, fc, :], pb[4 * grp + fc][:], AF.Silu, [Pb[4 * grp + fc]], [b_sgb[par]])
                    TT("dve", actT[par][:, fc, :], sgb[par][:, fc, :], pb[4 * grp + 2 + fc][:], ALU.mult,
                       [b_sgb[par], Pb[4 * grp + 2 + fc]], [b_actT[par]])

            def DOWN(idx):
                e, tt = steps[idx]
                grp = idx % 2
                par = idx % 2
                dn = wsl[:, a + 1, 0:2048].rearrange("p (f d) -> p f d", f=2)
                for st in range(4):
                    tile_i = tt * 4 + st
                    for hb in range(2):
                        bk = 4 * grp + 2 * (st % 2) + hb
                        for fc in range(2):
                            MM(pb[bk][:], actT[par][:, fc, st * 128:(st + 1) * 128], dn[:, fc, hb * 512:(hb + 1) * 512],
                               fc == 0, fc == 1, [b_actT[par], b_wsl[a + 1]], bk)
                        TT("dve", acc[tile_i][:, hb * 512:(hb + 1) * 512], pb[bk][:], acc[tile_i][:, hb * 512:(hb + 1) * 512],
                           ALU.add, [Pb[bk], b_acc[tile_i]], [b_acc[tile_i]])

            GU(0)
            for idx in range(len(steps)):
                if idx + 1 < len(steps):
                    GU(idx + 1)
                DOWN(idx)
            cbanks = {}

            def CG(i):
                tix = seq * 16 + i
                W = wsp[i % 2]
                CP("dve", W["bf"], wk_all[:, tix, :], [b_wk], [W["B"]])
                CP("dve", W["hi"], W["bf"], [W["B"]], [W["B"]])
                TT("dve", W["lo"], wk_all[:, tix, :], W["hi"], ALU.subtract, [b_wk, W["B"]], [W["B"]])
                bks = [nb(), nb()]
                cbanks[i] = bks
                for k in range(8):
                    gi = g_ctr[0] % 6
                    g_ctr[0] += 1
                    S.op("pool", lambda e_, gi=gi, tix=tix, k=k: e_.indirect_dma_start(
                        out=gbuf[gi], out_offset=None, in_=Ys,
                        in_offset=bass.IndirectOffsetOnAxis(ap=dst_all[:, tix, k:k + 1], axis=0)),
                        reads=[b_dst], writes=[b_gbuf[gi]], dma=f"d_g{gi}")
                    for part, nm in enumerate(("hi", "lo")):
                        di = dg_ctr[0] % 4
                        dg_ctr[0] += 1
                        TS("dve", dg[di], ident_bf[:], W[nm][:, k:k + 1], None, ALU.mult, None, [b_const, W["B"]], [b_dg[di]])
                        for hb in range(2):
                            MM(pb[bks[hb]][:], dg[di], gbuf[gi][:, hb * 512:(hb + 1) * 512], k == 0 and part == 0,
                               k == 7 and part == 1, [b_dg[di], b_gbuf[gi]], bks[hb])

            def CL(i):
                t0 = seq * SEQ + i * 128
                bks = cbanks[i]
                for hb in range(2):
                    TT("dve", acc[i][:, hb * 512:(hb + 1) * 512], pb[bks[hb]][:], acc[i][:, hb * 512:(hb + 1) * 512],
                       ALU.add, [Pb[bks[hb]], b_acc[i]], [b_acc[i]])
                layer_norm(acc[i], b_acc[i], acc[i], b_acc[i], ln2v[:, 0, :], ln2v[:, 1, :], b_ln2v, ss_m)
                DMA("act", out_d[t0:t0 + 128, :], acc[i], [b_acc[i]], [], f"d_out{i}")

            valid = [i for i in range(16) if seq * 16 + i < n_tiles * 2]
            for j, i in enumerate(valid):
                if j == 0:
                    CG(i)
                if j + 1 < len(valid):
                    CG(valid[j + 1])
                CL(i)

        sems = {k: es.enter_context(nc.semaphore(k)) for k in S.sem_keys()}
        finals = [(k, v) for k, v in S.dma_cnt.items() if k.startswith("d_out")]
        S.emit(nc, sems, finals)
    return nc, S


def _prep_shared(inp):
    f = np.float32
    c = lambda a: np.ascontiguousarray(a, dtype=f)
    lnv = np.stack([inp["ln_in_g"], inp["ln_in_b"], inp["gmlp_norm_g"][0], inp["gmlp_norm_b"][0],
                    inp["ln1_g"][0], inp["ln1_b"][0], inp["ln2_g"][0], inp["ln2_b"][0]], axis=0)
    lnvec = c(np.broadcast_to(lnv[None], (128, 8, D)))
    col = lambda v: np.asarray(v).reshape(8, 128).T
    convw = np.asarray(inp["conv_w"][0]).reshape(4, 8, 128).transpose(2, 0, 1).reshape(128, 32)
    cvec = c(np.concatenate([convw, col(inp["conv_b"][0]), col(inp["lru_ba"][0]), col(inp["lru_bi"][0]),
                             col(inp["lru_lambda"][0])], axis=1))
    wspT = c(np.asarray(inp["gmlp_spatial_w"][0]).transpose(2, 0, 1))
    maskT = c(np.triu(np.ones((128, 128), f)))
    shared = {
        "lnvec": lnvec, "cvec": cvec, "wspT": wspT, "maskT": maskT,
        "bsp": c(np.asarray(inp["gmlp_spatial_b"][0]).reshape(1, 1024)),
        "wa": c(np.asarray(inp["lru_wa"][0]).transpose(1, 0, 2)),
        "wi": c(np.asarray(inp["lru_wi"][0]).transpose(1, 0, 2)),
        "wr": c(np.asarray(inp["w_router"][0]).reshape(8, 128, 64).transpose(1, 0, 2)),
        "rbias": c(np.broadcast_to(np.asarray(inp["router_bias"][0])[None], (128, 64))),
        "ident": c(np.eye(128)),
        "iotaC": c(np.broadcast_to((np.arange(64, dtype=f) * CAP)[None], (128, 64))),
        "ustrict": c(np.triu(np.ones((128, 128), f), 1)),
        "dummyidx": c((NSLOT + np.arange(128, dtype=f)).reshape(128, 1)),
        "kvals": c(np.broadcast_to(np.arange(1, 9, dtype=f)[None], (128, 8))),
        "w_in": c(inp["w_in"][0]), "w_kv": c(inp["w_kv"][0]),
        "w_br0": c(inp["w_br_gmlp"][0]), "w_br1": c(inp["w_br_lru"][0]), "w_br2": c(inp["w_br_xa"][0]),
        "w_out": c(inp["w_out"][0]),
        "exp_gate": c(inp["exp_gate"][0]), "exp_up": c(inp["exp_up"][0]), "exp_down": c(inp["exp_down"][0]),
        "sh_gate": c(inp["sh_gate"][0]), "sh_up": c(inp["sh_up"][0]), "sh_down": c(inp["sh_down"][0]),
    }
    return shared


def kernel(**inputs):
    inp = {k: np.asarray(v) for k, v in inputs.items()}
    shared = _prep_shared(inp)
    x = np.ascontiguousarray(inp["x"], dtype=np.float32)
    mem = np.ascontiguousarray(inp["mem"], dtype=np.float32)
    nc, _ = build()
    in_maps = []
    for c in range(NCORES):
        m = dict(shared)
        m["x"] = x[2 * c:2 * c + 2].reshape(TOK, D)
        m["mem"] = mem[2 * c:2 * c + 2].reshape(512, D)
        in_maps.append(m)
    res = run_bass_kernel_spmd(nc, in_maps, core_ids=list(range(NCORES)))
    out = np.concatenate([np.asarray(r["out"]).reshape(2, SEQ, D) for r in res.results], axis=0)
    return out.astype(np.float32)
```
